# Optimizing a Trainium2 kernel written in Bass

```python
import math
import jax
import jax.numpy as jnp
from jax import lax
import numpy as np

D_MODEL = 2048
BATCH = 16
SEQ = 2048
DEPTH = 2

N_MIXERS = 2
N_ATTN_LAYERS = (DEPTH + 1) // 2
N_MLSTM_LAYERS = DEPTH // 2

DILATED_PATTERNS = ((128, 1), (512, 4), (2048, 16))
N_DIL_GROUPS = 3
ATTN_HEADS = 8
HEAD_DIM = 128
ROT_DIM = HEAD_DIM // 4
ROPE_THETA = 500000.0
ATTN_BLOCK = 128
ATTN_QKV_COLS = N_DIL_GROUPS * 3 * ATTN_HEADS * HEAD_DIM

MLSTM_HEADS = 8
QK_DIM = 128
V_DIM = D_MODEL // MLSTM_HEADS
CONV_K = 4
MLSTM_CHUNK = 64
MLSTM_QK_COLS = 2 * MLSTM_HEADS * QK_DIM
MLSTM_V_COLS = MLSTM_HEADS * V_DIM
MLSTM_IN_COLS = MLSTM_QK_COLS + 2 * MLSTM_V_COLS + 2 * MLSTM_HEADS

N_EXPERTS = 16
N_EXPERT_GROUPS = 4
EXPERTS_PER_GROUP = N_EXPERTS // N_EXPERT_GROUPS
TOP_K = 2
D_EXPERT = 768
MOE_BLOCK = 128

ALPHA = (2 * DEPTH) ** 0.25
BETA = (8 * DEPTH) ** -0.25
LN_EPS = 1e-5
NEG = -1e30

kernel_name = 'hybrid_dilated_attn_mlstm_grouped_moe'


def layer_norm(x, g, b):
    xf = x.astype(jnp.float32)
    mu = xf.mean(-1, keepdims=True)
    var = jnp.square(xf - mu).mean(-1, keepdims=True)
    return ((xf - mu) * lax.rsqrt(var + LN_EPS) * g + b).astype(x.dtype)


def rope_tables(seq):
    inv_freq = ROPE_THETA ** (-jnp.arange(0, ROT_DIM, 2, dtype=jnp.float32) / ROT_DIM)
    ang = jnp.arange(seq, dtype=jnp.float32)[:, None] * inv_freq[None, :]
    ang = jnp.concatenate([ang, ang], -1)
    return jnp.cos(ang)[:, None, :], jnp.sin(ang)[:, None, :]


def apply_partial_rope(t, cos, sin):
    tr, tp = t[..., :ROT_DIM], t[..., ROT_DIM:]
    x1, x2 = tr[..., :ROT_DIM // 2], tr[..., ROT_DIM // 2:]
    rot = jnp.concatenate([-x2, x1], -1)
    tr = (tr * cos + rot * sin).astype(t.dtype)
    return jnp.concatenate([tr, tp], -1)


def banded_causal_attention(q, k, v, n_back):
    n, h, l, hd = q.shape
    nb = -(-l // ATTN_BLOCK)
    pad = nb * ATTN_BLOCK - l
    padw = ((0, 0), (0, 0), (0, pad), (0, 0))
    qb, kb, vb = (jnp.pad(t, padw).reshape(n, h, nb, ATTN_BLOCK, hd) for t in (q, k, v))

    def with_prev(t):
        prev = jnp.concatenate([jnp.zeros_like(t[:, :, :1]), t[:, :, :-1]], axis=2)
        return jnp.concatenate([prev, t], axis=3)

    kc, vc = with_prev(kb), with_prev(vb)
    s = jnp.einsum('nhbqd,nhbkd->nhbqk', qb, kc).astype(jnp.float32) * (hd ** -0.5)
    blk = jnp.arange(nb)[:, None, None]
    qpos = blk * ATTN_BLOCK + jnp.arange(ATTN_BLOCK)[None, :, None]
    kpos = (blk - 1) * ATTN_BLOCK + jnp.arange(2 * ATTN_BLOCK)[None, None, :]
    dist = qpos - kpos
    mask = (dist >= 0) & (dist <= n_back) & (kpos >= 0)
    s = jnp.where(mask, s, NEG)
    mx = s.max(-1, keepdims=True)
    p = jnp.exp(s - mx)
    den = p.sum(-1)
    o = jnp.einsum('nhbqk,nhbkd->nhbqd', p.astype(v.dtype), vc).astype(jnp.float32) / den[..., None]
    lse = mx[..., 0] + jnp.log(den)
    o = o.reshape(n, h, nb * ATTN_BLOCK, hd)[:, :, :l]
    lse = lse.reshape(n, h, nb * ATTN_BLOCK)[:, :, :l]
    return o, lse


def strided_window_attention(q, k, v, n_back, dilation):
    b, s, h, hd = q.shape
    l = s // dilation

    def gather(t):
        return t.reshape(b, l, dilation, h, hd).transpose(0, 2, 3, 1, 4).reshape(b * dilation, h, l, hd)

    o, lse = banded_causal_attention(gather(q), gather(k), gather(v), n_back)
    o = o.reshape(b, dilation, h, l, hd).transpose(0, 3, 1, 2, 4).reshape(b, s, h, hd)
    lse = lse.reshape(b, dilation, h, l).transpose(0, 3, 1, 2).reshape(b, s, h)
    return o, lse


def dilated_attention(x, w_qkv, w_o, cos, sin):
    b, s, _ = x.shape
    qkv = (x @ w_qkv).reshape(b, s, N_DIL_GROUPS, 3, ATTN_HEADS, HEAD_DIM)
    outs, lses = [], []
    for g, (window, dil) in enumerate(DILATED_PATTERNS):
        q = apply_partial_rope(qkv[:, :, g, 0], cos, sin)
        k = apply_partial_rope(qkv[:, :, g, 1], cos, sin)
        o, lse = strided_window_attention(q, k, qkv[:, :, g, 2], window // dil, dil)
        outs.append(o)
        lses.append(lse)
    wts = jax.nn.softmax(jnp.stack(lses), axis=0)
    o = jnp.einsum('gbsh,gbshd->bshd', wts, jnp.stack(outs))
    return o.reshape(b, s, ATTN_HEADS * HEAD_DIM).astype(x.dtype) @ w_o


def causal_depthwise_conv(t, w, bias):
    s = t.shape[1]
    tp = jnp.pad(t, ((0, 0), (CONV_K - 1, 0), (0, 0)))
    out = bias
    for j in range(CONV_K):
        out = out + tp[:, j:j + s] * w[j]
    return out


def mlstm_cell(q, k, v, log_i, log_f):
    b, s, h, dqk = q.shape
    dv = v.shape[-1]
    nc = s // MLSTM_CHUNK
    lc = MLSTM_CHUNK

    def chunks(t):
        return t.astype(jnp.float32).reshape((b, nc, lc, h) + t.shape[3:]).swapaxes(0, 1).swapaxes(2, 3)

    xs = (chunks(q), chunks(k), chunks(v), chunks(log_i), chunks(log_f))
    causal = jnp.tril(jnp.ones((lc, lc), dtype=bool))

    def step(carry, inp):
        c_state, n_state, m_state = carry
        qc, kc, vc, ic, fc = inp
        bcum = jnp.cumsum(fc, axis=-1)
        dmat = jnp.where(causal, bcum[..., :, None] - bcum[..., None, :] + ic[..., None, :], NEG)
        inter = bcum + m_state[..., None]
        m_t = jnp.maximum(inter, dmat.max(-1))
        a = jnp.einsum('bhtd,bhsd->bhts', qc, kc) * jnp.exp(dmat - m_t[..., None])
        w_inter = jnp.exp(inter - m_t)
        num = jnp.einsum('bhts,bhsv->bhtv', a, vc) + w_inter[..., None] * jnp.einsum('bhtd,bhdv->bhtv', qc, c_state)
        den = a.sum(-1) + w_inter * jnp.einsum('bhtd,bhd->bht', qc, n_state)
        h_out = num / jnp.maximum(jnp.abs(den), jnp.exp(-m_t))[..., None]
        b_last = bcum[..., -1]
        g = b_last[..., None] - bcum + ic
        m_new = jnp.maximum(b_last + m_state, g.max(-1))
        wk = jnp.exp(g - m_new[..., None])
        decay = jnp.exp(b_last + m_state - m_new)
        c_new = decay[..., None, None] * c_state + jnp.einsum('bhs,bhsd,bhsv->bhdv', wk, kc, vc)
        n_new = decay[..., None] * n_state + jnp.einsum('bhs,bhsd->bhd', wk, kc)
        return (c_new, n_new, m_new), h_out

    init = (jnp.zeros((b, h, dqk, dv), jnp.float32),
            jnp.zeros((b, h, dqk), jnp.float32),
            jnp.full((b, h), NEG, jnp.float32))
    _, hs = lax.scan(step, init, xs)
    return hs.swapaxes(2, 3).swapaxes(0, 1).reshape(b, s, h, dv)


def mlstm_mixer(x, w_in, b_gates, conv_w, conv_b, norm_g, w_out):
    b, s, _ = x.shape
    z = x @ w_in
    qk = jax.nn.silu(causal_depthwise_conv(z[..., :MLSTM_QK_COLS], conv_w, conv_b))
    half = MLSTM_QK_COLS // 2
    q = qk[..., :half].reshape(b, s, MLSTM_HEADS, QK_DIM)
    k = qk[..., half:].reshape(b, s, MLSTM_HEADS, QK_DIM) * (QK_DIM ** -0.5)
    v = z[..., MLSTM_QK_COLS:MLSTM_QK_COLS + MLSTM_V_COLS].reshape(b, s, MLSTM_HEADS, V_DIM)
    o_pre = z[..., MLSTM_QK_COLS + MLSTM_V_COLS:MLSTM_QK_COLS + 2 * MLSTM_V_COLS]
    gates = z[..., MLSTM_QK_COLS + 2 * MLSTM_V_COLS:].astype(jnp.float32) + b_gates
    log_i = gates[..., :MLSTM_HEADS]
    log_f = jax.nn.log_sigmoid(gates[..., MLSTM_HEADS:])
    hc = mlstm_cell(q, k, v, log_i, log_f)
    mu = hc.mean(-1, keepdims=True)
    var = jnp.square(hc - mu).mean(-1, keepdims=True)
    hn = (hc - mu) * lax.rsqrt(var + LN_EPS) * norm_g.reshape(MLSTM_HEADS, V_DIM)
    y = hn.reshape(b, s, MLSTM_V_COLS).astype(x.dtype) * jax.nn.sigmoid(o_pre)
    return y @ w_out


def grouped_moe(x, w_router, b_router, w_gate, w_up, w_down):
    b, s, d = x.shape
    xt = x.reshape(-1, d)
    t = xt.shape[0]
    logits = (xt @ w_router).astype(jnp.float32) + b_router
    probs = jax.nn.softmax(logits, axis=-1).reshape(t, N_EXPERT_GROUPS, EXPERTS_PER_GROUP)
    top_p, top_i = lax.top_k(probs, TOP_K)
    g_sel = jnp.argmax(top_p.sum(-1), axis=-1)
    sel_p = jnp.take_along_axis(top_p, g_sel[:, None, None], axis=1)[:, 0]
    sel_i = jnp.take_along_axis(top_i, g_sel[:, None, None], axis=1)[:, 0]
    gate = sel_p / sel_p.sum(-1, keepdims=True)
    expert = g_sel[:, None] * EXPERTS_PER_GROUP + sel_i
    a = t * TOP_K
    e_flat = expert.reshape(a)
    tok = jnp.repeat(jnp.arange(t), TOP_K)
    counts = jnp.zeros((N_EXPERTS,), jnp.int32).at[e_flat].add(1)
    padded = (counts + MOE_BLOCK - 1) // MOE_BLOCK * MOE_BLOCK
    ends = jnp.cumsum(padded)
    starts = ends - padded
    order = jnp.argsort(e_flat)
    e_sorted = e_flat[order]
    rank = jnp.arange(a) - (jnp.cumsum(counts) - counts)[e_sorted]
    dest = jnp.zeros((a,), jnp.int32).at[order].set(starts[e_sorted] + rank)
    rows = a + N_EXPERTS * MOE_BLOCK
    buf = jnp.zeros((rows, d), x.dtype).at[dest].set(xt[tok])
    n_blocks = rows // MOE_BLOCK
    block_expert = jnp.minimum(jnp.searchsorted(ends, jnp.arange(n_blocks) * MOE_BLOCK, side='right'), N_EXPERTS - 1)

    def expert_block(args):
        xb, e = args
        hb = jax.nn.silu(xb @ w_gate[e]) * (xb @ w_up[e])
        return hb @ w_down[e]

    out = lax.map(expert_block, (buf.reshape(n_blocks, MOE_BLOCK, d), block_expert)).reshape(rows, d)
    y = (out[dest].reshape(t, TOP_K, d) * gate[..., None].astype(x.dtype)).sum(1)
    return y.reshape(b, s, d)


def setup_inputs(seed: int = 0) -> dict:
    key = jax.random.key(seed)
    ks = jax.random.split(key, 24)

    def nrm(k, shape, scale):
        return jax.random.normal(k, shape, jnp.float32) * scale

    x = nrm(ks[0], (BATCH, SEQ, D_MODEL), 1.0)
    qkv_scale = jnp.array([1.0, 1.0, BETA], jnp.float32)[None, None, None, :, None]
    attn_w_qkv = (nrm(ks[1], (N_ATTN_LAYERS, D_MODEL, N_DIL_GROUPS, 3, ATTN_HEADS * HEAD_DIM), D_MODEL ** -0.5)
                  * qkv_scale).reshape(N_ATTN_LAYERS, D_MODEL, ATTN_QKV_COLS)
    attn_w_o = nrm(ks[2], (N_ATTN_LAYERS, ATTN_HEADS * HEAD_DIM, D_MODEL), (ATTN_HEADS * HEAD_DIM) ** -0.5 * BETA)
    col_scale = jnp.concatenate([jnp.ones((MLSTM_QK_COLS,), jnp.float32),
                                 jnp.full((MLSTM_V_COLS,), BETA, jnp.float32),
                                 jnp.ones((MLSTM_V_COLS + 2 * MLSTM_HEADS,), jnp.float32)])
    mlstm_w_in = nrm(ks[3], (N_MLSTM_LAYERS, D_MODEL, MLSTM_IN_COLS), D_MODEL ** -0.5) * col_scale
    i_bias = nrm(ks[4], (N_MLSTM_LAYERS, MLSTM_HEADS), 0.1)
    f_bias = jnp.linspace(3.0, 6.0, MLSTM_HEADS, dtype=jnp.float32)[None, :] + nrm(ks[5], (N_MLSTM_LAYERS, MLSTM_HEADS), 0.1)
    mlstm_b_gates = jnp.concatenate([i_bias, f_bias], axis=-1)
    mlstm_conv_w = nrm(ks[6], (N_MLSTM_LAYERS, CONV_K, MLSTM_QK_COLS), CONV_K ** -0.5)
    mlstm_conv_b = nrm(ks[7], (N_MLSTM_LAYERS, MLSTM_QK_COLS), 0.02)
    mlstm_norm_g = 1.0 + nrm(ks[8], (N_MLSTM_LAYERS, MLSTM_V_COLS), 0.02)
    mlstm_w_out = nrm(ks[9], (N_MLSTM_LAYERS, MLSTM_V_COLS, D_MODEL), MLSTM_V_COLS ** -0.5 * BETA)
    ln_mix_g = 1.0 + nrm(ks[10], (DEPTH, D_MODEL), 0.02)
    ln_mix_b = nrm(ks[11], (DEPTH, D_MODEL), 0.02)
    ln_ffn_g = 1.0 + nrm(ks[12], (DEPTH, D_MODEL), 0.02)
    ln_ffn_b = nrm(ks[13], (DEPTH, D_MODEL), 0.02)
    router_w = nrm(ks[14], (D_MODEL, N_EXPERTS), D_MODEL ** -0.5)
    router_b = nrm(ks[15], (N_EXPERTS,), 0.01)
    moe_w_gate = nrm(ks[16], (DEPTH, N_EXPERTS, D_MODEL, D_EXPERT), D_MODEL ** -0.5)
    moe_w_up = nrm(ks[17], (DEPTH, N_EXPERTS, D_MODEL, D_EXPERT), D_MODEL ** -0.5 * BETA)
    moe_w_down = nrm(ks[18], (DEPTH, N_EXPERTS, D_EXPERT, D_MODEL), D_EXPERT ** -0.5 * BETA)
    return {'x': x, 'attn_w_qkv': attn_w_qkv, 'attn_w_o': attn_w_o,
            'mlstm_w_in': mlstm_w_in, 'mlstm_b_gates': mlstm_b_gates,
            'mlstm_conv_w': mlstm_conv_w, 'mlstm_conv_b': mlstm_conv_b,
            'mlstm_norm_g': mlstm_norm_g, 'mlstm_w_out': mlstm_w_out,
            'ln_mix_g': ln_mix_g, 'ln_mix_b': ln_mix_b, 'ln_ffn_g': ln_ffn_g, 'ln_ffn_b': ln_ffn_b,
            'router_w': router_w, 'router_b': router_b,
            'moe_w_gate': moe_w_gate, 'moe_w_up': moe_w_up, 'moe_w_down': moe_w_down}


def reference(x, attn_w_qkv, attn_w_o, mlstm_w_in, mlstm_b_gates, mlstm_conv_w, mlstm_conv_b,
              mlstm_norm_g, mlstm_w_out, ln_mix_g, ln_mix_b, ln_ffn_g, ln_ffn_b,
              router_w, router_b, moe_w_gate, moe_w_up, moe_w_down):
    cos, sin = rope_tables(x.shape[1])
    cos, sin = cos.astype(x.dtype), sin.astype(x.dtype)
    for i in range(DEPTH):
        j = i // N_MIXERS
        if i % N_MIXERS == 0:
            mixed = dilated_attention(x, attn_w_qkv[j], attn_w_o[j], cos, sin)
        else:
            mixed = mlstm_mixer(x, mlstm_w_in[j], mlstm_b_gates[j], mlstm_conv_w[j], mlstm_conv_b[j],
                                mlstm_norm_g[j], mlstm_w_out[j])
        x = layer_norm(ALPHA * x + mixed, ln_mix_g[i], ln_mix_b[i])
        ffn = grouped_moe(x, router_w, router_b, moe_w_gate[i], moe_w_up[i], moe_w_down[i])
        x = layer_norm(ALPHA * x + ffn, ln_ffn_g[i], ln_ffn_b[i])
    return x
```

```python
import numpy as np
from contextlib import ExitStack
import concourse.bass as bass
import concourse.mybir as mybir
from concourse.bass_utils import run_bass_kernel_spmd

F32 = mybir.dt.float32
BF16 = mybir.dt.bfloat16
AF = mybir.ActivationFunctionType
ALU = mybir.AluOpType
AX = mybir.AxisListType

NTOK = 4096
D = 2048
ALPHA = 4.0 ** 0.25
EPS_LN = 1e-5 / (ALPHA * ALPHA)
NEG = -1e30


class Tile:
    def __init__(self, t):
        self.t = t
        self.w = None
        self.r = {}

    def __getitem__(self, k):
        return self.t[k]


class Sched:
    def __init__(self, nc, st):
        self.nc = nc
        self.eng = {'pe': nc.tensor, 'act': nc.scalar, 'dve': nc.vector, 'pool': nc.gpsimd, 'sp': nc.sync}
        self.sem = {k: st.enter_context(nc.semaphore('s_' + k)) for k in self.eng}
        self.cnt = {k: 0 for k in self.eng}
        self.ND = 24
        self.dsem = [st.enter_context(nc.semaphore('d%d' % i)) for i in range(self.ND)]
        self.dcnt = [0] * self.ND
        self.dnext = 0
        self.NSW = 4
        self.swnext = 0
        self.known = {k: {} for k in self.eng}
        self.uid = 0

    def sb(self, st, shape, dt, name=None):
        self.uid += 1
        return Tile(st.enter_context(self.nc.sbuf_tensor('%s_%d' % (name or 'sb', self.uid), list(shape), dt)))

    def ps(self, st, shape, dt, name=None):
        self.uid += 1
        return Tile(st.enter_context(self.nc.psum_tensor('%s_%d' % (name or 'ps', self.uid), list(shape), dt)))

    def _semof(self, key):
        return self.sem[key] if isinstance(key, str) else self.dsem[key[1]]

    def _wait(self, eng, deps):
        kn = self.known[eng]
        for key, val in deps.items():
            if kn.get(key, 0) >= val:
                continue
            self.eng[eng].wait_ge(self._semof(key), val)
            kn[key] = val

    def _deps(self, eng, reads, writes):
        d = {}

        def add(k, v):
            if d.get(k, 0) < v:
                d[k] = v
        for t in reads:
            if t.w is not None:
                add(*t.w)
        for t in writes:
            if t.w is not None:
                if not (eng == 'pe' and t.w[0] == 'pe'):
                    add(*t.w)
            for k, v in t.r.items():
                add(k, v)
        return d

    def op(self, eng, fn, reads=(), writes=()):
        self._wait(eng, self._deps(eng, reads, writes))
        ins = fn(self.eng[eng])
        self.cnt[eng] += 1
        n = self.cnt[eng]
        ins.then_inc(self.sem[eng], 1)
        for t in reads:
            t.r[eng] = n
        for t in writes:
            t.w = (eng, n)
            t.r = {}

    def dma(self, out, in_, reads=(), writes=(), q='sp'):
        if q == 'pool':
            j = self.ND - self.NSW + self.swnext
            self.swnext = (self.swnext + 1) % self.NSW
        else:
            j = self.dnext
            self.dnext = (j + 1) % (self.ND - self.NSW)
        d = self._deps(q, reads, writes)
        if self.dcnt[j] > 0:
            k = ('d', j)
            d[k] = max(d.get(k, 0), self.dcnt[j])
        self._wait(q, d)
        self.dcnt[j] += 16
        self.eng[q].dma_start(out=out, in_=in_).then_inc(self.dsem[j], 16)
        k = ('d', j)
        for t in reads:
            t.r[k] = self.dcnt[j]
        for t in writes:
            t.w = (k, self.dcnt[j])
            t.r = {}

    def barrier(self):
        d = {k: v for k, v in self.cnt.items() if v > 0}
        for j in range(self.ND):
            if self.dcnt[j] > 0:
                d[('d', j)] = self.dcnt[j]
        for e in self.eng:
            self._wait(e, dict(d))


class Rot:
    def __init__(self, tiles):
        self.tiles = tiles
        self.i = 0

    def next(self):
        t = self.tiles[self.i % len(self.tiles)]
        self.i += 1
        return t


FULL = dict(nseq=2, nhead=8, nB=16, nln=32, ntt=8, nexp=16, ncc=16, nblk=16, nch=32, ncellh=8)


def build(upto=99, taps=False, cfg=None):
    C = dict(FULL)
    C.update(cfg or {})
    nc = bass.Bass("TRN2", target_bir_lowering=False)

    def din(name, shape, dt=F32):
        return nc.dram_tensor(name, list(shape), dt, kind="ExternalInput").ap()

    def dscr(name, shape, dt):
        return nc.dram_tensor(name, list(shape), dt, kind=("ExternalOutput" if taps else "Internal")).ap()

    x_in = din("x", [NTOK, D])
    w_qkv = din("attn_w_qkv", [D, 9216])
    w_o = din("attn_w_o", [1024, D])
    w_in = din("mlstm_w_in", [D, 6160])
    b_gates = din("mlstm_b_gates", [16, 1])
    conv_w = din("mlstm_conv_w", [4, 2048])
    conv_b = din("mlstm_conv_b", [1, 2048])
    norm_g = din("mlstm_norm_g", [1, 2048])
    w_out = din("mlstm_w_out", [D, D])
    ln_g = [din("ln_mix_g", [2, D]), din("ln_ffn_g", [2, D])]
    ln_b = [din("ln_mix_b", [2, D]), din("ln_ffn_b", [2, D])]
    router_w = din("router_w", [D, 16])
    router_b = din("router_b", [1, 16])
    NE = C.get("ne_decl", 16)
    moe_wg = din("moe_w_gate", [2, NE, D, 768])
    moe_wu = din("moe_w_up", [2, NE, D, 768])
    moe_wd = din("moe_w_down", [2, NE, 768, D])
    c_amask = din("c_amask", [128, 23 * 128])
    c_rope = din("c_rope", [32, 2, 2048])
    c_pm = din("c_pm", [32, 32])
    c_mt = din("c_mt", [64, 64])
    c_sel = din("c_sel", [8, 8, 128])
    y_out = nc.dram_tensor("y", [NTOK, D], F32, kind="ExternalOutput").ap()

    XT = dscr("XT", [D, NTOK], BF16)
    XRa = dscr("XRa", [NTOK, D], F32)
    XRb = dscr("XRb", [NTOK, D], F32)
    OT = dscr("OT", [D, NTOK], BF16)
    QKT = dscr("QKT", [D, NTOK], BF16)
    VV = dscr("VV", [NTOK, D], BF16)
    OG = dscr("OG", [NTOK, D], BF16)

    gst = ExitStack()
    S = Sched(nc, gst)

    ident = S.sb(gst, [128, 128], F32, 'ident')
    identb = S.sb(gst, [128, 128], BF16, 'identb')
    Gt = S.sb(gst, [128, 32, 16], F32, 'G')
    wr = S.sb(gst, [128, 16, 16], F32, 'wr')
    brb = S.sb(gst, [128, 16], F32, 'brb')
    mhalf = S.sb(gst, [128, 1], F32, 'mhalf')
    gbc = S.sb(gst, [128, D], F32, 'gbc')
    bbc = S.sb(gst, [128, D], F32, 'bbc')
    sm = S.sb(gst, [128, 256], F32, 'sm')

    S.op('pool', lambda e: e.memset(ident.t[:], 1.0), writes=[ident])
    S.op('pool', lambda e: e.affine_select(out=ident.t[:], in_=ident.t[:], pattern=[[-1, 128]],
                                           compare_op=ALU.is_equal, fill=0.0, base=0, channel_multiplier=1),
         reads=[ident], writes=[ident])
    S.op('pool', lambda e: e.tensor_copy(identb.t[:], ident.t[:]), reads=[ident], writes=[identb])
    S.op('pool', lambda e: e.memset(mhalf.t[:], -0.5), writes=[mhalf])
    S.dma(wr.t[:], router_w.rearrange("(c p) n -> p c n", p=128), writes=[wr])
    S.dma(brb.t[:], router_b.partition_broadcast(128), writes=[brb])
    wrh = S.sb(gst, [128, 16, 16], BF16, 'wrh')
    wrl = S.sb(gst, [128, 16, 16], BF16, 'wrl')
    S.op('dve', lambda e: e.tensor_copy(wrh.t[:], wr.t[:]), reads=[wr], writes=[wrh])
    S.op('dve', lambda e: e.tensor_tensor(wrl.t[:], wr.t[:], wrh.t[:], ALU.subtract), reads=[wr, wrh], writes=[wrl])

    def load_gb(which, layer):
        S.dma(gbc.t[:], ln_g[which][layer:layer + 1, :].partition_broadcast(128), writes=[gbc])
        S.dma(bbc.t[:], ln_b[which][layer:layer + 1, :].partition_broadcast(128), writes=[bbc])

    cast_rr = [0]

    def wload(stage_rot, dst_tile, dst_ap, src_ap, shape):
        stg = stage_rot.next()
        if len(shape) == 3:
            sap = stg.t[:, 0:shape[1] * shape[2]].rearrange("p (a b) -> p a b", a=shape[1])
        else:
            sap = stg.t[:, 0:shape[1]]
        S.dma(sap, src_ap, writes=[stg])
        eng = ('pool', 'pool', 'act')[cast_rr[0] % 3]
        cast_rr[0] += 1
        if eng == 'act':
            S.op('act', lambda e: e.copy(dst_ap, sap), reads=[stg], writes=[dst_tile])
        else:
            S.op(eng, lambda e: e.tensor_copy(dst_ap, sap), reads=[stg], writes=[dst_tile])

    def tstore(src, nblk, DST, row0, tok0, psl, xt, is_f32, xtf=None):
        per = 4 if is_f32 else 8
        idt = ident if is_f32 else identb
        ngrp = (nblk + per - 1) // per
        used = []
        for g in range(ngrp):
            p = psl[g % len(psl)]
            n = min(per, nblk - g * per)

            def f(e, g=g, p=p, n=n):
                for k in range(n):
                    c = g * per + k
                    ins = e.transpose(p.t[:, k * 128:(k + 1) * 128], src.t[:, c * 128:(c + 1) * 128], idt.t[:])
                return ins
            S.op('pe', f, reads=[src, idt], writes=[p])
            used.append((p, g, n))

        def ev(e):
            for (p, g, n) in used:
                ins = e.copy(xt.t[:, g * per:g * per + n, :],
                             p.t[:, 0:n * 128].rearrange("p (k t) -> p k t", k=n))
            return ins
        S.op('act', ev, reads=[u[0] for u in used], writes=[xt])
        if xtf is not None:
            xf = xtf[0]

            def ev2(e):
                for (p, g, n) in used:
                    ins = e.copy(xf.t[:, g * per:g * per + n, :],
                                 p.t[:, 0:n * 128].rearrange("p (k t) -> p k t", k=n))
                return ins
            S.op('act', ev2, reads=[u[0] for u in used], writes=[xf])
        S.dma(DST[row0:row0 + nblk * 128, tok0:tok0 + 128].rearrange("(c p) t -> p c t", p=128),
              xt.t[:, 0:nblk, :], reads=[xt])

    def ln_tile(t, ti, Xout, psl, xt, xtf=None, psr=None, do_xt=True):
        tok0 = ti * 128
        st6 = sm.t[:, 0:24].rearrange("p (a b) -> p a b", a=4)
        mv = sm.t[:, 24:26]
        rs = sm.t[:, 26:27]

        def f1(e):
            for k in range(4):
                ins = e.bn_stats(st6[:, k, :], t.t[:, k * 512:(k + 1) * 512])
            return ins
        S.op('dve', f1, reads=[t], writes=[sm])
        S.op('dve', lambda e: e.bn_aggr(mv, sm.t[:, 0:24]), reads=[sm], writes=[sm])
        S.op('pool', lambda e: e.tensor_scalar(rs, sm.t[:, 25:26], EPS_LN, None, ALU.add), reads=[sm], writes=[sm])
        S.op('pool', lambda e: e.tensor_tensor(rs, rs, mhalf.t[:], ALU.pow), reads=[sm, mhalf], writes=[sm])
        S.op('dve', lambda e: e.tensor_scalar(t.t[:], t.t[:], sm.t[:, 24:25], rs, ALU.subtract, ALU.mult),
             reads=[t, sm], writes=[t])
        S.op('dve', lambda e: e.tensor_tensor(t.t[:], t.t[:], gbc.t[:], ALU.mult), reads=[t, gbc], writes=[t])
        S.op('dve', lambda e: e.tensor_tensor(t.t[:], t.t[:], bbc.t[:], ALU.add), reads=[t, bbc], writes=[t])
        S.dma(Xout[tok0:tok0 + 128, :], t.t[:], reads=[t])
        if do_xt:
            tstore(t, 16, XT, 0, tok0, psl, xt, True, xtf)
        if xtf is not None:
            router(ti, xtf, psr, xt)

    def router(ti, xtf_, psr, xt):
        xtf, xl = xtf_
        rst = C.get('rstage', 9)
        if rst < 1:
            return
        S.op('dve', lambda e: e.tensor_tensor(xl.t[:], xtf.t[:], xt.t[:], ALU.subtract), reads=[xtf, xt], writes=[xl])

        def f(e):
            k = 0
            for c in range(16):
                for (a, b) in ((xt, wrh), (xt, wrl), (xl, wrh)):
                    ins = e.matmul(psr.t[:, 0:16], a.t[:, c, :], b.t[:, c, :], start=(k == 0), stop=(k == 47))
                    k += 1
            return ins
        if rst < 2:
            return
        S.op('pe', f, reads=[xt, xl, wrh, wrl], writes=[psr])
        if rst < 3:
            return
        lg = sm.t[:, 32:48]
        pr = sm.t[:, 48:64]
        pr3 = pr.rearrange("p (a b) -> p a b", a=4)
        eq1 = sm.t[:, 64:80]
        eq13 = eq1.rearrange("p (a b) -> p a b", a=4)
        pr2 = sm.t[:, 80:96]
        pr23 = pr2.rearrange("p (a b) -> p a b", a=4)
        eq2 = sm.t[:, 96:112]
        eq23 = eq2.rearrange("p (a b) -> p a b", a=4)
        m1 = sm.t[:, 112:116]
        m2 = sm.t[:, 116:120]
        sc = sm.t[:, 120:124]
        geq = sm.t[:, 124:128]
        mx = sm.t[:, 128:129]
        ssum = sm.t[:, 129:130]
        gm = sm.t[:, 130:131]
        den = sm.t[:, 131:132]
        def G4(a, g):
            return a[:, 4 * g:4 * g + 4]
        seq = [
            ('dve', lambda e: e.tensor_tensor(lg, psr.t[:, 0:16], brb.t[:], ALU.add)),
            ('dve', lambda e: e.reduce_max(mx, lg, AX.X)),
            ('dve', lambda e: e.tensor_scalar(mx, mx, -1.0, None, ALU.mult)),
            ('act', lambda e: e.activation(pr, lg, AF.Exp, bias=mx, scale=1.0)),
            ('dve', lambda e: e.reduce_sum(ssum, pr, AX.X)),
            ('dve', lambda e: e.reciprocal(ssum, ssum)),
            ('dve', lambda e: e.tensor_scalar(pr, pr, ssum, None, ALU.mult)),
        ]
        for g in range(4):
            seq.append(('dve', lambda e, g=g: e.reduce_max(m1[:, g:g + 1], G4(pr, g), AX.X)))
        for g in range(4):
            seq.append(('dve', lambda e, g=g: e.tensor_scalar(G4(eq1, g), G4(pr, g), m1[:, g:g + 1], None, ALU.is_ge)))
        seq.append(('dve', lambda e: e.scalar_tensor_tensor(pr2, eq1, -2.0, pr, ALU.mult, ALU.add)))
        for g in range(4):
            seq.append(('dve', lambda e, g=g: e.reduce_max(m2[:, g:g + 1], G4(pr2, g), AX.X)))
        for g in range(4):
            seq.append(('dve', lambda e, g=g: e.tensor_scalar(G4(eq2, g), G4(pr2, g), m2[:, g:g + 1], None, ALU.is_ge)))
        seq += [
            ('dve', lambda e: e.tensor_tensor(sc, m1, m2, ALU.add)),
            ('dve', lambda e: e.reduce_max(gm, sc, AX.X)),
            ('dve', lambda e: e.tensor_scalar(geq, sc, gm, None, ALU.is_ge)),
            ('dve', lambda e: e.tensor_tensor(eq1, eq1, eq2, ALU.add)),
        ]
        for g in range(4):
            seq.append(('dve', lambda e, g=g: e.tensor_scalar(G4(eq1, g), G4(eq1, g), geq[:, g:g + 1], None, ALU.mult)))
        seq += [
            ('dve', lambda e: e.tensor_tensor(pr, pr, eq1, ALU.mult)),
            ('dve', lambda e: e.reduce_sum(den, pr, AX.X)),
            ('dve', lambda e: e.reciprocal(den, den)),
            ('dve', lambda e: e.tensor_scalar(Gt.t[:, ti, :], pr, den, 1.0 / ALPHA, ALU.mult, ALU.mult)),
        ]
        for i, (eng, fn) in enumerate(seq):
            rd = [sm, psr, brb] if i == 0 else [sm]
            wt = [sm, Gt] if i == len(seq) - 1 else [sm]
            S.op(eng, fn, reads=rd, writes=wt)

    def phase_x2xt():
        with ExitStack() as st:
            xs = Rot([S.sb(st, [128, D], F32) for _ in range(2)])
            xts = Rot([S.sb(st, [128, 16, 128], BF16) for _ in range(2)])
            psl = [S.ps(st, [128, 512], F32) for _ in range(4)]
            for i in range(32):
                x = xs.next()
                S.dma(x.t[:], x_in[i * 128:(i + 1) * 128, :], writes=[x])
                tstore(x, 16, XT, 0, i * 128, psl, xts.next(), True)
            S.barrier()

    GD = [1, 4, 15]
    MOFF = [0, 2, 7]

    def phase_attn():
        with ExitStack() as st:
            stage = Rot([S.sb(st, [128, 2048], F32) for _ in range(1)])
            wq = [S.sb(st, [128, 16, 128], BF16) for _ in range(9)]
            xtl = Rot([S.sb(st, [128, 16, 512], BF16) for _ in range(1)])
            QK = [[S.sb(st, [128, 2048], BF16) for _ in range(2)] for _ in range(3)]
            Vt = [S.sb(st, [128, 16, 128], BF16) for _ in range(3)]
            amask = S.sb(st, [128, 23 * 128], F32)
            rope = S.sb(st, [32, 2, 2048], F32)
            pmf = S.sb(st, [32, 32], F32)
            pm = S.sb(st, [32, 32], BF16)
            SsbR = [S.sb(st, [128, 23 * 128], F32) for _ in range(2)]
            PbR = [S.sb(st, [128, 23 * 128], BF16) for _ in range(2)]
            PTR = [S.sb(st, [128, 23 * 128], BF16) for _ in range(2)]
            smsR = [S.sb(st, [128, 8], F32) for _ in range(2)]
            osb = Rot([S.sb(st, [128, 128], BF16) for _ in range(2)])
            r1 = S.sb(st, [32, 512], F32)
            r2 = S.sb(st, [32, 512], F32)
            ots = Rot([S.sb(st, [128, 128], BF16) for _ in range(2)])
            psA = Rot([S.ps(st, [128, 512], F32) for _ in range(3)])
            psT = Rot([S.ps(st, [128, 1024], BF16) for _ in range(3)])
            psO = Rot([S.ps(st, [128, 512], F32) for _ in range(2)])
            S.dma(amask.t[:], c_amask, writes=[amask])
            S.dma(rope.t[:], c_rope, writes=[rope])
            S.dma(pmf.t[:], c_pm, writes=[pmf])
            S.op('pool', lambda e: e.tensor_copy(pm.t[:], pmf.t[:]), reads=[pmf], writes=[pm])
            scale = 128.0 ** -0.5
            def load_w(h):
                for g in range(3):
                    for j in range(3):
                        wt = wq[g * 3 + j]
                        col = g * 3072 + j * 1024 + h * 128
                        wload(stage, wt, wt.t[:], w_qkv[:, col:col + 128].rearrange("(c p) n -> p c n", p=128),
                              [128, 16, 128])
            sh_list = [(s, h) for s in range(C['nseq']) for h in range(C['nhead'])]
            load_w(sh_list[0][1])
            for si, (s, h) in enumerate(sh_list):
                if True:
                    for tt in range(4):
                        xt_ = xtl.next()
                        S.dma(xt_.t[:], XT[:, s * 2048 + tt * 512: s * 2048 + (tt + 1) * 512]
                              .rearrange("(c p) t -> p c t", p=128), writes=[xt_])
                        cs = slice(tt * 512, (tt + 1) * 512)
                        for g in range(3):
                            for j in range(3):
                                wt = wq[g * 3 + j]
                                p = psA.next()
                                if j < 2:
                                    def f(e, p=p, wt=wt, xt_=xt_):
                                        for c in range(16):
                                            ins = e.matmul(p.t[:], wt.t[:, c, :], xt_.t[:, c, :],
                                                           start=(c == 0), stop=(c == 15))
                                        return ins
                                    S.op('pe', f, reads=[wt, xt_], writes=[p])
                                    dst = QK[g][j]
                                    S.op('act', lambda e, p=p, dst=dst, j=j: e.activation(
                                        dst.t[:, cs], p.t[:], AF.Copy, scale=(scale if j == 0 else 1.0)),
                                        reads=[p], writes=[dst])
                                    p2 = psO.next()
                                    S.op('pe', lambda e, p2=p2, dst=dst: e.matmul(
                                        p2.t[0:32, :], pm.t[:], dst.t[0:32, cs], start=True, stop=True),
                                        reads=[pm, dst], writes=[p2])
                                    S.op('dve', lambda e, dst=dst: e.tensor_tensor(
                                        r1.t[:], dst.t[0:32, cs], rope.t[:, 0, cs], ALU.mult),
                                        reads=[dst, rope], writes=[r1])
                                    S.op('dve', lambda e, p2=p2: e.tensor_tensor(
                                        r2.t[:], p2.t[0:32, :], rope.t[:, 1, cs], ALU.mult),
                                        reads=[p2, rope], writes=[r2])
                                    S.op('dve', lambda e, dst=dst: e.tensor_tensor(
                                        dst.t[0:32, cs], r1.t[:], r2.t[:], ALU.add),
                                        reads=[r1, r2], writes=[dst])
                                else:
                                    def f(e, p=p, wt=wt, xt_=xt_):
                                        for b in range(4):
                                            for c in range(16):
                                                ins = e.matmul(p.t[:, b * 128:(b + 1) * 128],
                                                               xt_.t[:, c, b * 128:(b + 1) * 128], wt.t[:, c, :],
                                                               start=(c == 0), stop=(c == 15))
                                        return ins
                                    S.op('pe', f, reads=[wt, xt_], writes=[p])
                                    S.op('act', lambda e, p=p, g=g: e.copy(
                                        Vt[g].t[:, tt * 4:(tt + 1) * 4, :],
                                        p.t[:].rearrange("p (b n) -> p b n", b=4)), reads=[p], writes=[Vt[g]])
                    if si + 1 < len(sh_list):
                        load_w(sh_list[si + 1][1])

                    def stage1(B):
                        Ssb = SsbR[B % 2]
                        col = 0
                        for g in range(3):
                            jb = max(0, B - GD[g])
                            while jb <= B:
                                n = min(4, B + 1 - jb)
                                p = psA.next()
                                S.op('pe', lambda e, p=p, g=g, jb=jb, n=n: e.matmul(
                                    p.t[:, 0:n * 128], QK[g][0].t[:, B * 128:(B + 1) * 128],
                                    QK[g][1].t[:, jb * 128:(jb + n) * 128], start=True, stop=True),
                                    reads=[QK[g][0], QK[g][1]], writes=[p])
                                mo = (MOFF[g] + GD[g] - B + jb) * 128
                                S.op('dve', lambda e, p=p, n=n, col=col, mo=mo: e.tensor_tensor(
                                    Ssb.t[:, col:col + n * 128], p.t[:, 0:n * 128], amask.t[:, mo:mo + n * 128], ALU.add),
                                    reads=[p, amask], writes=[Ssb])
                                col += n * 128
                                jb += n

                    def stage2(B):
                        Ssb, Pb, PT, sms = SsbR[B % 2], PbR[B % 2], PTR[B % 2], smsR[B % 2]
                        kb = []
                        for g in range(3):
                            for jb in range(max(0, B - GD[g]), B + 1):
                                kb.append((g, jb))
                        nb = len(kb)
                        nc_ = nb * 128
                        S.op('dve', lambda e: e.reduce_max(sms.t[:, 0:1], Ssb.t[:, 0:nc_], AX.X),
                             reads=[Ssb], writes=[sms])
                        S.op('dve', lambda e: e.tensor_scalar(sms.t[:, 1:2], sms.t[:, 0:1], -1.0, None, ALU.mult),
                             reads=[sms], writes=[sms])
                        S.op('act', lambda e: e.activation(Pb.t[:, 0:nc_], Ssb.t[:, 0:nc_], AF.Exp,
                                                           bias=sms.t[:, 1:2], scale=1.0, accum_out=sms.t[:, 2:3]),
                             reads=[Ssb, sms], writes=[Pb, sms])
                        S.op('dve', lambda e: e.reciprocal(sms.t[:, 3:4], sms.t[:, 2:3]), reads=[sms], writes=[sms])
                        i = 0
                        while i < nb:
                            n = min(8, nb - i)
                            pt = psT.next()

                            def f(e, pt=pt, i=i, n=n):
                                for k in range(n):
                                    ins = e.transpose(pt.t[:, k * 128:(k + 1) * 128],
                                                      Pb.t[:, (i + k) * 128:(i + k + 1) * 128], identb.t[:])
                                return ins
                            S.op('pe', f, reads=[Pb, identb], writes=[pt])
                            if (i // 8) % 2 == 0:
                                S.op('act', lambda e, pt=pt, i=i, n=n: e.copy(
                                    PT.t[:, i * 128:(i + n) * 128], pt.t[:, 0:n * 128]), reads=[pt], writes=[PT])
                            else:
                                S.op('dve', lambda e, pt=pt, i=i, n=n: e.tensor_copy(
                                    PT.t[:, i * 128:(i + n) * 128], pt.t[:, 0:n * 128]), reads=[pt], writes=[PT])
                            i += n
                        po = psO.next()

                        def f(e, po=po, kb=kb, nb=nb):
                            for i, (g, jb) in enumerate(kb):
                                ins = e.matmul(po.t[:, 0:128], PT.t[:, i * 128:(i + 1) * 128], Vt[g].t[:, jb, :],
                                               start=(i == 0), stop=(i == nb - 1))
                            return ins
                        S.op('pe', f, reads=[PT] + Vt, writes=[po])
                        ob = osb.next()
                        S.op('act', lambda e, po=po, ob=ob: e.activation(
                            ob.t[:], po.t[:, 0:128], AF.Copy, scale=sms.t[:, 3:4]),
                            reads=[po, sms], writes=[ob])
                        pt = psT.next()
                        S.op('pe', lambda e, pt=pt, ob=ob: e.transpose(pt.t[:, 0:128], ob.t[:], identb.t[:]),
                             reads=[ob, identb], writes=[pt])
                        ot = ots.next()
                        S.op('act', lambda e, pt=pt, ot=ot: e.copy(ot.t[:], pt.t[:, 0:128]), reads=[pt], writes=[ot])
                        t0 = s * 2048 + B * 128
                        S.dma(OT[h * 128:(h + 1) * 128, t0:t0 + 128], ot.t[:], reads=[ot])
                    nB = C['nB']
                    stage1(0)
                    for B in range(nB):
                        if B + 1 < nB:
                            stage1(B + 1)
                        stage2(B)
            S.barrier()

    def tstore_view(tile, view, nblk, DST, row0, tok0, psl, xt):
        p = psl[(tok0 // 128) % len(psl)]

        def f(e):
            for k in range(nblk):
                ins = e.transpose(p.t[:, k * 128:(k + 1) * 128], view[:, k * 128:(k + 1) * 128], identb.t[:])
            return ins
        S.op('pe', f, reads=[tile, identb], writes=[p])
        S.op('act', lambda e: e.copy(xt.t[:, 0:nblk, :], p.t[:, 0:nblk * 128].rearrange("p (k t) -> p k t", k=nblk)),
             reads=[p], writes=[xt])
        S.dma(DST[row0:row0 + nblk * 128, tok0:tok0 + 128].rearrange("(c p) t -> p c t", p=128),
              xt.t[:, 0:nblk, :], reads=[xt])

    def phase_proj_ln(KC, Wd, INT, resid, which, layer, Xout, with_router):
        with ExitStack() as st:
            stage = Rot([S.sb(st, [128, 2048], F32) for _ in range(2)])
            W = S.sb(st, [128, KC, D], BF16)
            ins_ = Rot([S.sb(st, [128, KC, 128], BF16) for _ in range(2)])
            rt = Rot([S.sb(st, [128, D], F32) for _ in range(2)])
            xts = Rot([S.sb(st, [128, 16, 128], BF16) for _ in range(2)])
            xtf = (S.sb(st, [128, 16, 128], F32), S.sb(st, [128, 16, 128], BF16)) if with_router else None
            psl = [S.ps(st, [128, 512], F32) for _ in range(4)]
            psm = [S.ps(st, [128, 512], F32) for _ in range(3)]
            psr = S.ps(st, [128, 512], F32) if with_router else None
            load_gb(which, layer)
            for c in range(KC):
                wload(stage, W, W.t[:, c, :], Wd[c * 128:(c + 1) * 128, :], [128, D])
            for ti in range(C['nln']):
                it = ins_.next()
                S.dma(it.t[:], INT[0:KC * 128, ti * 128:(ti + 1) * 128].rearrange("(c p) t -> p c t", p=128),
                      writes=[it])
                t = rt.next()
                S.dma(t.t[:], resid[ti * 128:(ti + 1) * 128, :], writes=[t])
                for dc in range(4):
                    p = psm[dc % 3]

                    def f(e, p=p, dc=dc, it=it):
                        for c in range(KC):
                            ins = e.matmul(p.t[:], it.t[:, c, :], W.t[:, c, dc * 512:(dc + 1) * 512],
                                           start=(c == 0), stop=(c == KC - 1))
                        return ins
                    S.op('pe', f, reads=[it, W], writes=[p])
                    S.op('dve', lambda e, p=p, dc=dc, t=t: e.scalar_tensor_tensor(
                        t.t[:, dc * 512:(dc + 1) * 512], p.t[:], 1.0 / ALPHA, t.t[:, dc * 512:(dc + 1) * 512],
                        ALU.mult, ALU.add), reads=[p, t], writes=[t])
                ln_tile(t, ti, Xout, psl, xts.next(), xtf, psr)
            S.barrier()

    def phase_moe(layer, resid, Xout, final):
        with ExitStack() as st:
            stage = Rot([S.sb(st, [128, 2048], F32) for _ in range(3)])
            wgu = Rot([S.sb(st, [128, 16, 128], BF16) for _ in range(6)])
            wdl = Rot([S.sb(st, [128, 6, D], BF16) for _ in range(2)])
            xt_ = S.sb(st, [128, 16, 512], BF16)
            Y = [S.sb(st, [128, D], F32) for _ in range(4)]
            hT = [S.sb(st, [128, 512], BF16) for _ in range(6)]
            sg = Rot([S.sb(st, [128, 512], F32) for _ in range(2)])
            xts = Rot([S.sb(st, [128, 16, 128], BF16) for _ in range(2)])
            psG = Rot([S.ps(st, [128, 512], F32) for _ in range(2)])
            psU = Rot([S.ps(st, [128, 512], F32) for _ in range(2)])
            psD = Rot([S.ps(st, [128, 512], F32) for _ in range(4)])
            load_gb(1, layer)
            for tt in range(C['ntt']):
                S.dma(xt_.t[:], XT[:, tt * 512:(tt + 1) * 512].rearrange("(c p) t -> p c t", p=128), writes=[xt_])
                for b in range(4):
                    S.dma(Y[b].t[:], resid[tt * 512 + b * 128: tt * 512 + (b + 1) * 128, :], writes=[Y[b]])
                for e_ in range(C['nexp']):
                    wd = wdl.next()
                    S.dma(wd.t[:], moe_wd[layer, e_].rearrange("(c p) n -> p c n", p=128), writes=[wd], q='pool')
                    for fc in range(6):
                        wg = wgu.next()
                        wu = wgu.next()
                        wload(stage, wg, wg.t[:], moe_wg[layer, e_, :, fc * 128:(fc + 1) * 128]
                              .rearrange("(c p) n -> p c n", p=128), [128, 16, 128])
                        wload(stage, wu, wu.t[:], moe_wu[layer, e_, :, fc * 128:(fc + 1) * 128]
                              .rearrange("(c p) n -> p c n", p=128), [128, 16, 128])
                        pg = psG.next()
                        pu = psU.next()

                        def f(e, pg=pg, wg=wg):
                            for c in range(16):
                                ins = e.matmul(pg.t[:], wg.t[:, c, :], xt_.t[:, c, :], start=(c == 0), stop=(c == 15))
                            return ins
                        S.op('pe', f, reads=[wg, xt_], writes=[pg])

                        def f(e, pu=pu, wu=wu):
                            for c in range(16):
                                ins = e.matmul(pu.t[:], wu.t[:, c, :], xt_.t[:, c, :], start=(c == 0), stop=(c == 15))
                            return ins
                        S.op('pe', f, reads=[wu, xt_], writes=[pu])
                        sg_ = sg.next()
                        S.op('act', lambda e, pg=pg, sg_=sg_: e.activation(sg_.t[:], pg.t[:], AF.Silu),
                             reads=[pg], writes=[sg_])
                        S.op('dve', lambda e, pu=pu, sg_=sg_, fc=fc: e.tensor_tensor(
                            hT[fc].t[:], sg_.t[:], pu.t[:], ALU.mult), reads=[sg_, pu], writes=[hT[fc]])
                    for b in range(4):
                        for dc in range(4):
                            pd = psD.next()

                            def f(e, pd=pd, b=b, dc=dc, wd=wd):
                                for fc in range(6):
                                    ins = e.matmul(pd.t[:], hT[fc].t[:, b * 128:(b + 1) * 128],
                                                   wd.t[:, fc, dc * 512:(dc + 1) * 512],
                                                   start=(fc == 0), stop=(fc == 5))
                                return ins
                            S.op('pe', f, reads=hT + [wd], writes=[pd])
                            gi = tt * 4 + b
                            S.op('dve', lambda e, pd=pd, b=b, dc=dc, gi=gi, e_=e_: e.scalar_tensor_tensor(
                                Y[b].t[:, dc * 512:(dc + 1) * 512], pd.t[:], Gt.t[:, gi, e_:e_ + 1],
                                Y[b].t[:, dc * 512:(dc + 1) * 512], ALU.mult, ALU.add),
                                reads=[pd, Gt, Y[b]], writes=[Y[b]])
                for b in range(4):
                    ln_tile(Y[b], tt * 4 + b, Xout, psD.tiles, xts.next(), do_xt=(not final))
            S.barrier()

    GS = dscr("GS", [16, NTOK], F32)

    def phase_mlstm_proj():
        with ExitStack() as st:
            stage = Rot([S.sb(st, [128, 2048], F32) for _ in range(2)])
            xs = S.sb(st, [128, 16, 2048], BF16)
            zTR = Rot([S.sb(st, [128, 2051], F32) for _ in range(2)])
            accR = Rot([S.sb(st, [128, 2048], F32) for _ in range(2)])
            qkb = Rot([S.sb(st, [128, 2048], BF16) for _ in range(2)])
            wsm = Rot([S.sb(st, [128, 16, 128], BF16) for _ in range(2)])
            wbg = Rot([S.sb(st, [128, 16, 512], BF16) for _ in range(2)])
            wgt = S.sb(st, [128, 16, 16], BF16)
            vb = Rot([S.sb(st, [128, 512], BF16) for _ in range(3)])
            gsb = Rot([S.sb(st, [8, 512], F32) for _ in range(2)])
            cw = S.sb(st, [128, 4, 16], F32)
            cb = S.sb(st, [128, 16], F32)
            psA = Rot([S.ps(st, [128, 512], F32) for _ in range(4)])
            with nc.allow_non_contiguous_dma(reason="tiny conv weight transpose"):
                for j in range(4):
                    S.dma(cw.t[:, j, :], conv_w[j:j + 1, :].rearrange("o (c p) -> p (o c)", p=128), writes=[cw])
                S.dma(cb.t[:], conv_b.rearrange("o (c p) -> p (o c)", p=128), writes=[cb])
            for zT in zTR.tiles:
                S.op('pool', lambda e, zT=zT: e.memset(zT.t[:, 0:3], 0.0), writes=[zT])
            wload(stage, wgt, wgt.t[:], w_in[:, 6144:6160].rearrange("(c p) n -> p c n", p=128), [128, 16, 16])
            for s in range(C['nseq']):
                S.dma(xs.t[:], XT[:, s * 2048:(s + 1) * 2048].rearrange("(c p) t -> p c t", p=128), writes=[xs])
                for tt in range(4):
                    for half in range(2):
                        p = psA.next()

                        def f(e, p=p, half=half, tt=tt):
                            for c in range(16):
                                ins = e.matmul(p.t[0:8, :], wgt.t[:, c, half * 8:(half + 1) * 8],
                                               xs.t[:, c, tt * 512:(tt + 1) * 512], start=(c == 0), stop=(c == 15))
                            return ins
                        S.op('pe', f, reads=[wgt, xs], writes=[p])
                        gt_ = gsb.next()
                        S.op('act', lambda e, p=p, gt_=gt_: e.copy(gt_.t[:], p.t[0:8, :]), reads=[p], writes=[gt_])
                        S.dma(GS[half * 8:(half + 1) * 8, s * 2048 + tt * 512: s * 2048 + (tt + 1) * 512], gt_.t[:],
                              reads=[gt_])
                for cc in (range(16) if C['ncc'] == 16 else (0, 8)):
                    w = wsm.next()
                    zT = zTR.next()
                    acc = accR.next()
                    wload(stage, w, w.t[:], w_in[:, cc * 128:(cc + 1) * 128].rearrange("(c p) n -> p c n", p=128),
                          [128, 16, 128])
                    for tt in range(4):
                        p = psA.next()

                        def f(e, p=p, w=w, tt=tt):
                            for c in range(16):
                                ins = e.matmul(p.t[:], w.t[:, c, :], xs.t[:, c, tt * 512:(tt + 1) * 512],
                                               start=(c == 0), stop=(c == 15))
                            return ins
                        S.op('pe', f, reads=[w, xs], writes=[p])
                        S.op('act', lambda e, p=p, tt=tt, zT=zT: e.copy(zT.t[:, 3 + tt * 512: 3 + (tt + 1) * 512], p.t[:]),
                             reads=[p], writes=[zT])
                    S.op('dve', lambda e, cc=cc, zT=zT, acc=acc: e.tensor_scalar(acc.t[:], zT.t[:, 3:2051], cw.t[:, 3, cc:cc + 1],
                                                                cb.t[:, cc:cc + 1], ALU.mult, ALU.add),
                         reads=[zT, cw, cb], writes=[acc])
                    for j in (2, 1, 0):
                        S.op('dve', lambda e, cc=cc, j=j, zT=zT, acc=acc: e.scalar_tensor_tensor(
                            acc.t[:], zT.t[:, j:j + 2048], cw.t[:, j, cc:cc + 1], acc.t[:], ALU.mult, ALU.add),
                            reads=[zT, cw, acc], writes=[acc])
                    S.op('act', lambda e, acc=acc: e.activation(acc.t[:], acc.t[:], AF.Silu), reads=[acc], writes=[acc])
                    qb = qkb.next()
                    S.op('pool', lambda e, qb=qb, cc=cc, acc=acc: e.tensor_scalar(
                        qb.t[:], acc.t[:], (1.0 if cc < 8 else 128.0 ** -0.5), None, ALU.mult),
                        reads=[acc], writes=[qb])
                    S.dma(QKT[cc * 128:(cc + 1) * 128, s * 2048:(s + 1) * 2048], qb.t[:], reads=[qb])
                for nch in range(8):
                    w = wbg.next()
                    c0 = 2048 + nch * 512
                    for c4 in range(4):
                        wload(stage, w, w.t[:, c4 * 4:(c4 + 1) * 4, :],
                              w_in[c4 * 512:(c4 + 1) * 512, c0:c0 + 512].rearrange("(c p) n -> p c n", p=128),
                              [128, 4, 512])
                    for blk in range(C['nblk']):
                        p = psA.next()

                        def f(e, p=p, w=w, blk=blk):
                            for c in range(16):
                                ins = e.matmul(p.t[:], xs.t[:, c, blk * 128:(blk + 1) * 128], w.t[:, c, :],
                                               start=(c == 0), stop=(c == 15))
                            return ins
                        S.op('pe', f, reads=[w, xs], writes=[p])
                        v_ = vb.next()
                        r0 = s * 2048 + blk * 128
                        if nch < 4:
                            S.op('act', lambda e, p=p, v_=v_: e.copy(v_.t[:], p.t[:]), reads=[p], writes=[v_])
                            S.dma(VV[r0:r0 + 128, nch * 512:(nch + 1) * 512], v_.t[:], reads=[v_])
                        else:
                            S.op('act', lambda e, p=p, v_=v_: e.activation(v_.t[:], p.t[:], AF.Sigmoid),
                                 reads=[p], writes=[v_])
                            S.dma(OG[r0:r0 + 128, (nch - 4) * 512:(nch - 3) * 512], v_.t[:], reads=[v_])
            S.barrier()

    def phase_mlstm_cell():
        with ExitStack() as st:
            qk = S.sb(st, [128, 16, 2048], BF16)
            cols = S.sb(st, [128, 1024], F32)
            decb = S.sb(st, [128, 256], F32)
            selt = S.sb(st, [8, 8, 128], F32)
            m01 = S.sb(st, [64, 64], F32)
            bg = S.sb(st, [8, 2], F32)
            vx = Rot([S.sb(st, [64, 8, 257], BF16) for _ in range(2)])
            ogt = Rot([S.sb(st, [64, 2048], BF16) for _ in range(2)])
            Yt = Rot([S.sb(st, [64, 2048], BF16) for _ in range(2)])
            yT = Rot([S.sb(st, [128, 16, 64], BF16) for _ in range(2)])
            Cx = [S.sb(st, [128, 257], F32) for _ in range(8)]
            Cb = [S.sb(st, [128, 257], BF16) for _ in range(8)]
            aT = Rot([S.sb(st, [64, 64], BF16) for _ in range(3)])
            vp = Rot([S.sb(st, [64, 257], BF16) for _ in range(3)])
            p1s = Rot([S.sb(st, [64, 257], F32) for _ in range(3)])
            num = Rot([S.sb(st, [64, 257], F32) for _ in range(3)])
            hn = Rot([S.sb(st, [64, 256], F32) for _ in range(3)])
            sst = Rot([S.sb(st, [64, 16], F32) for _ in range(3)])
            kcs = Rot([S.sb(st, [64, 128], BF16) for _ in range(3)])
            psS = Rot([S.ps(st, [128, 512], F32) for _ in range(2)])
            psP = Rot([S.ps(st, [128, 512], F32) for _ in range(2)])
            psU = Rot([S.ps(st, [128, 512], F32) for _ in range(2)])
            psB = Rot([S.ps(st, [128, 1024], BF16) for _ in range(2)])
            S.dma(selt.t[:], c_sel, writes=[selt])
            S.dma(m01.t[:], c_mt, writes=[m01])
            S.op('dve', lambda e: e.tensor_scalar(m01.t[:], m01.t[:], 0.0, None, ALU.is_equal), reads=[m01], writes=[m01])
            S.dma(bg.t[:, 0:1], b_gates[0:8, :], writes=[bg])
            S.dma(bg.t[:, 1:2], b_gates[8:16, :], writes=[bg])
            S.dma(gbc.t[:], norm_g.partition_broadcast(128), writes=[gbc])
            for v_ in vx.tiles:
                S.op('pool', lambda e, v_=v_: e.memset(v_.t[:], 1.0), writes=[v_])
            for s in range(C['nseq']):
                S.dma(qk.t[:], QKT[:, s * 2048:(s + 1) * 2048].rearrange("(c p) t -> p c t", p=128), writes=[qk])
                with ExitStack() as st2:
                    def g8():
                        return S.sb(st2, [8, 2048], F32)
                    gi, gf, ta, tb, Bc, Wc, wv = g8(), g8(), g8(), g8(), g8(), g8(), g8()
                    small = S.sb(st2, [8, 256], F32)
                    mst = small.t[:, 0:33]
                    mul = small.t[:, 40:72]
                    dec = small.t[:, 80:112]
                    S.dma(gi.t[:], GS[0:8, s * 2048:(s + 1) * 2048], writes=[gi])
                    S.dma(gf.t[:], GS[8:16, s * 2048:(s + 1) * 2048], writes=[gf])

                    def v3(t_, sl=None):
                        a = t_.t[:, :].rearrange("p (c l) -> p c l", l=64)
                        return a

                    def D_(fn, rd, wt):
                        S.op('dve', fn, reads=rd, writes=wt)

                    def A_(fn, rd, wt):
                        S.op('act', fn, reads=rd, writes=wt)
                    D_(lambda e: e.tensor_scalar(gi.t[:], gi.t[:], bg.t[:, 0:1], None, ALU.add), [gi, bg], [gi])
                    D_(lambda e: e.tensor_scalar(gf.t[:], gf.t[:], bg.t[:, 1:2], None, ALU.add), [gf, bg], [gf])
                    D_(lambda e: e.scalar_tensor_tensor(ta.t[:], gf.t[:], -1.0, gf.t[:], ALU.mult, ALU.max), [gf], [ta])
                    A_(lambda e: e.activation(ta.t[:], ta.t[:], AF.Exp, scale=-1.0), [ta], [ta])
                    A_(lambda e: e.activation(ta.t[:], ta.t[:], AF.Ln, bias=1.0), [ta], [ta])
                    D_(lambda e: e.tensor_scalar(tb.t[:], gf.t[:], 0.0, None, ALU.min), [gf], [tb])
                    D_(lambda e: e.tensor_tensor(tb.t[:], tb.t[:], ta.t[:], ALU.subtract), [tb, ta], [tb])

                    def scan(src, dst, tmp, op):
                        cur, nxt = src, dst
                        for k in (1, 2, 4, 8, 16, 32):
                            c3, n3 = v3(cur), v3(nxt)
                            D_(lambda e, c3=c3, n3=n3, k=k: e.tensor_tensor(n3[:, :, k:], c3[:, :, k:], c3[:, :, :64 - k], op),
                               [cur], [nxt])
                            S.op('pool', lambda e, c3=c3, n3=n3, k=k: e.tensor_copy(n3[:, :, :k], c3[:, :, :k]), reads=[cur], writes=[nxt])
                            cur, nxt = nxt, (tmp if nxt is dst else dst)
                        return cur
                    Bres = scan(tb, Bc, ta, ALU.add)
                    D_(lambda e: e.tensor_tensor(wv.t[:], gi.t[:], Bres.t[:], ALU.subtract), [gi, Bres], [wv])
                    free = [t_ for t_ in (ta, tb, Bc) if t_ is not Bres]
                    Wres = scan(wv, free[0], free[1], ALU.max)
                    B3, W3 = v3(Bres), v3(Wres)
                    D_(lambda e: e.memset(small.t[:, 0:1], NEG), [], [small])
                    for c in range(32):
                        D_(lambda e, c=c: e.tensor_tensor(small.t[:, 40 + c:41 + c], small.t[:, c:c + 1],
                                                          W3[:, c, 63:64], ALU.max), [small, Wres], [small])
                        D_(lambda e, c=c: e.tensor_tensor(small.t[:, c + 1:c + 2], B3[:, c, 63:64],
                                                          small.t[:, 40 + c:41 + c], ALU.add), [small, Bres], [small])
                    D_(lambda e: e.tensor_tensor(dec, small.t[:, 0:32], mul, ALU.subtract), [small], [small])
                    A_(lambda e: e.activation(dec, dec, AF.Exp), [small], [small])
                    mb = small.t[:, 0:32].unsqueeze(2).broadcast_to([8, 32, 64])
                    mulb = mul.unsqueeze(2).broadcast_to([8, 32, 64])
                    D_(lambda e: e.tensor_tensor(W3, W3, mb, ALU.max), [Wres, small], [Wres])
                    mu = Wres
                    q_wk, q_rf, q_wi, q_th = gi, gf, free[0] if free[0] is not Wres else free[1], None
                    others = [t_ for t_ in (ta, tb, Bc) if t_ is not Bres and t_ is not Wres]
                    q_wi = others[0]
                    D_(lambda e: e.tensor_tensor(v3(q_wk), v3(wv), mulb, ALU.subtract), [wv, small], [q_wk])
                    A_(lambda e: e.activation(q_wk.t[:], q_wk.t[:], AF.Exp), [q_wk], [q_wk])
                    D_(lambda e: e.tensor_tensor(v3(q_rf), mulb, v3(mu), ALU.subtract), [mu, small], [q_rf])
                    A_(lambda e: e.activation(q_rf.t[:], q_rf.t[:], AF.Exp), [q_rf], [q_rf])
                    D_(lambda e: e.tensor_tensor(v3(q_wi), mb, v3(mu), ALU.subtract), [mu, small], [q_wi])
                    A_(lambda e: e.activation(q_wi.t[:], q_wi.t[:], AF.Exp), [q_wi], [q_wi])
                    D_(lambda e: e.tensor_tensor(wv.t[:], Bres.t[:], mu.t[:], ALU.add), [Bres, mu, wv], [wv])
                    A_(lambda e: e.activation(wv.t[:], wv.t[:], AF.Exp, scale=-1.0), [wv], [wv])
                    q_th = wv
                    for qi, Q in enumerate((q_wk, q_rf, q_wi, q_th)):
                        p = psS.next()

                        def f(e, p=p, Q=Q):
                            for c in range(32):
                                ins = e.transpose(p.t[0:64, c * 8:(c + 1) * 8], Q.t[0:8, c * 64:(c + 1) * 64],
                                                  ident.t[0:8, 0:8])
                            return ins
                        S.op('pe', f, reads=[Q, ident], writes=[p])
                        S.op('act', lambda e, p=p, qi=qi: e.copy(cols.t[0:64, qi * 256:(qi + 1) * 256], p.t[0:64, 0:256]),
                             reads=[p], writes=[cols])
                    p = psS.next()

                    def f(e, p=p):
                        for h in range(8):
                            ins = e.matmul(p.t[:, h * 32:(h + 1) * 32], selt.t[0:8, h, :], small.t[0:8, 80:112],
                                           start=True, stop=True)
                        return ins
                    S.op('pe', f, reads=[selt, small], writes=[p])
                    S.op('act', lambda e, p=p: e.copy(decb.t[:], p.t[:, 0:256]), reads=[p], writes=[decb])
                    S.barrier()
                for h in range(8):
                    S.op('pool', lambda e, h=h: e.memset(Cx[h].t[:], 0.0), writes=[Cx[h]])
                    S.op('pool', lambda e, h=h: e.memset(Cb[h].t[:], 0.0), writes=[Cb[h]])
                for c in range(C['nch']):
                    r0 = s * 2048 + c * 64
                    vx_ = vx.next()
                    S.dma(vx_.t[:, :, 0:256], VV[r0:r0 + 64, :].rearrange("t (h v) -> t h v", h=8), writes=[vx_])
                    og_ = ogt.next()
                    S.dma(og_.t[:], OG[r0:r0 + 64, :], writes=[og_])
                    yt = Yt.next()
                    csl = slice(c * 64, (c + 1) * 64)
                    for h in range(C['ncellh']):
                        def col(qi):
                            o = qi * 256 + c * 8 + h
                            return cols.t[0:64, o:o + 1]
                        pa = psS.next()
                        S.op('pe', lambda e, pa=pa, h=h: e.matmul(pa.t[0:64, 0:64], qk.t[:, 8 + h, csl], qk.t[:, h, csl],
                                                                  start=True, stop=True), reads=[qk], writes=[pa])
                        a_ = aT.next()
                        S.op('dve', lambda e, pa=pa, a_=a_: e.tensor_tensor(a_.t[:], pa.t[0:64, 0:64], m01.t[:], ALU.mult),
                             reads=[pa, m01], writes=[a_])
                        vp_ = vp.next()
                        S.op('pool', lambda e, vp_=vp_, h=h, vx_=vx_: e.tensor_scalar(
                            vp_.t[:], vx_.t[:, h, :], col(0), None, ALU.mult), reads=[vx_, cols], writes=[vp_])
                        p1 = psP.next()
                        S.op('pe', lambda e, p1=p1, a_=a_, vp_=vp_: e.matmul(p1.t[0:64, 0:257], a_.t[:], vp_.t[:],
                                                                            start=True, stop=True),
                             reads=[a_, vp_], writes=[p1])
                        p2 = psP.next()
                        S.op('pe', lambda e, p2=p2, h=h: e.matmul(p2.t[0:64, 0:257], qk.t[:, h, csl], Cb[h].t[:],
                                                                  start=True, stop=True),
                             reads=[qk, Cb[h]], writes=[p2])
                        p1s_ = p1s.next()
                        S.op('act', lambda e, p1=p1, p1s_=p1s_: e.activation(p1s_.t[:], p1.t[0:64, 0:257], AF.Copy,
                                                                              scale=col(1)),
                             reads=[p1, cols], writes=[p1s_])
                        n_ = num.next()
                        S.op('dve', lambda e, p2=p2, p1s_=p1s_, n_=n_: e.scalar_tensor_tensor(
                            n_.t[:], p2.t[0:64, 0:257], col(2), p1s_.t[:], ALU.mult, ALU.add),
                            reads=[p2, p1s_, cols], writes=[n_])
                        ss = sst.next()

                        S.op('dve', lambda e, n_=n_, ss=ss: e.scalar_tensor_tensor(
                            ss.t[:, 11:12], n_.t[:, 256:257], -1.0, n_.t[:, 256:257], ALU.mult, ALU.max), reads=[n_], writes=[ss])
                        S.op('dve', lambda e, ss=ss: e.tensor_tensor(
                            ss.t[:, 0:1], ss.t[:, 11:12], col(3), ALU.max), reads=[ss, cols], writes=[ss])
                        S.op('dve', lambda e, ss=ss: e.reciprocal(ss.t[:, 1:2], ss.t[:, 0:1]), reads=[ss], writes=[ss])
                        h_ = hn.next()
                        S.op('dve', lambda e, n_=n_, ss=ss, h_=h_: e.tensor_scalar(
                            h_.t[:], n_.t[:, 0:256], ss.t[:, 1:2], None, ALU.mult), reads=[n_, ss], writes=[h_])

                        S.op('dve', lambda e, h_=h_, ss=ss: e.bn_stats(ss.t[:, 2:8], h_.t[:]), reads=[h_, ss], writes=[ss])
                        S.op('dve', lambda e, ss=ss: e.bn_aggr(ss.t[:, 8:10], ss.t[:, 2:8]), reads=[ss], writes=[ss])
                        S.op('pool', lambda e, ss=ss: e.tensor_scalar(ss.t[:, 10:11], ss.t[:, 9:10], 1e-5, None, ALU.add),
                             reads=[ss], writes=[ss])
                        S.op('pool', lambda e, ss=ss: e.tensor_tensor(ss.t[:, 10:11], ss.t[:, 10:11], mhalf.t[0:64, :], ALU.pow),
                             reads=[ss, mhalf], writes=[ss])
                        S.op('dve', lambda e, h_=h_, ss=ss: e.tensor_scalar(
                            h_.t[:], h_.t[:], ss.t[:, 8:9], ss.t[:, 10:11], ALU.subtract, ALU.mult),
                            reads=[h_, ss], writes=[h_])
                        hs = slice(h * 256, (h + 1) * 256)
                        S.op('pool', lambda e, h_=h_: e.tensor_tensor(h_.t[:], h_.t[:], gbc.t[0:64, hs], ALU.mult),
                             reads=[h_, gbc], writes=[h_])
                        S.op('pool', lambda e, h_=h_, yt=yt, og_=og_: e.tensor_tensor(
                            yt.t[:, hs], h_.t[:], og_.t[:, hs], ALU.mult), reads=[h_, og_], writes=[yt])
                        pk = psB.next()
                        S.op('pe', lambda e, pk=pk, h=h: e.transpose(pk.t[0:64, 0:128], qk.t[:, 8 + h, csl], identb.t[:]),
                             reads=[qk, identb], writes=[pk])
                        k_ = kcs.next()
                        S.op('act', lambda e, pk=pk, k_=k_: e.copy(k_.t[:], pk.t[0:64, 0:128]), reads=[pk], writes=[k_])
                        pu = psU.next()
                        S.op('pe', lambda e, pu=pu, k_=k_, vp_=vp_: e.matmul(pu.t[:, 0:257], k_.t[:], vp_.t[:],
                                                                            start=True, stop=True),
                             reads=[k_, vp_], writes=[pu])
                        S.op('dve', lambda e, pu=pu, h=h: e.scalar_tensor_tensor(
                            Cx[h].t[:], Cx[h].t[:], decb.t[:, h * 32 + c: h * 32 + c + 1], pu.t[:, 0:257],
                            ALU.mult, ALU.add), reads=[Cx[h], decb, pu], writes=[Cx[h]])
                        S.op('act', lambda e, h=h: e.copy(Cb[h].t[:], Cx[h].t[:]), reads=[Cx[h]], writes=[Cb[h]])
                    pk = psB.next()

                    def f(e, pk=pk, yt=yt):
                        for k in range(16):
                            ins = e.transpose(pk.t[:, k * 64:(k + 1) * 64], yt.t[:, k * 128:(k + 1) * 128],
                                              identb.t[0:64, 0:64])
                        return ins
                    S.op('pe', f, reads=[yt, identb], writes=[pk])
                    y_ = yT.next()
                    S.op('act', lambda e, pk=pk, y_=y_: e.copy(y_.t[:], pk.t[:, 0:1024].rearrange("p (k t) -> p k t", k=16)),
                         reads=[pk], writes=[y_])
                    S.dma(OT[:, r0:r0 + 64].rearrange("(c p) t -> p c t", p=128), y_.t[:], reads=[y_])
            S.barrier()

    phase_x2xt()
    if upto >= 1:
        phase_attn()
    if upto >= 2:
        phase_proj_ln(8, w_o, OT, x_in, 0, 0, XRa, not C.get('norouter'))
    if upto >= 3:
        phase_moe(0, XRa, XRb, False)
    if upto >= 4:
        phase_mlstm_proj()
    if upto >= 5:
        phase_mlstm_cell()
    if upto >= 6:
        phase_proj_ln(16, w_out, OT, XRb, 0, 1, XRa, True)
    if upto >= 7:
        phase_moe(1, XRa, y_out, True)
    S.barrier()
    gst.close()
    return nc


def make_consts():
    am = np.full((128, 23, 128), NEG, np.float32)
    q = np.arange(128)[:, None]
    k = np.arange(128)[None, :]
    blk = 0
    for g, (win, dil, dmax) in enumerate(((128, 1, 1), (512, 4, 4), (2048, 16, 15))):
        for delta in range(dmax, -1, -1):
            dist = 128 * delta + q - k
            ok = (dist >= 0) & (dist <= win) & (dist % dil == 0)
            am[:, blk, :] = np.where(ok, 0.0, NEG)
            blk += 1
    inv = (500000.0 ** (-np.arange(0, 32, 2, dtype=np.float32) / 32)).astype(np.float32)
    ang = np.arange(2048, dtype=np.float32)[None, :] * np.concatenate([inv, inv])[:, None]
    rope = np.zeros((32, 2, 2048), np.float32)
    rope[:, 0, :] = np.cos(ang)
    sn = np.sin(ang)
    sn[:16] = -sn[:16]
    rope[:, 1, :] = sn
    pm = np.zeros((32, 32), np.float32)
    for i in range(32):
        pm[(i + 16) % 32, i] = 1.0
    mt = np.where(np.arange(64)[:, None] <= np.arange(64)[None, :], 0.0, NEG).astype(np.float32)
    sel = np.zeros((8, 8, 128), np.float32)
    for h in range(8):
        sel[h, h, :] = 1.0
    return {"c_amask": am.reshape(128, 23 * 128), "c_rope": rope, "c_pm": pm, "c_mt": mt, "c_sel": sel}


def make_in_maps(inp, ncores=8):
    f = lambda a: np.ascontiguousarray(a, dtype=np.float32)
    shared = {
        "attn_w_qkv": f(inp["attn_w_qkv"][0]), "attn_w_o": f(inp["attn_w_o"][0]),
        "mlstm_w_in": f(inp["mlstm_w_in"][0]), "mlstm_b_gates": f(inp["mlstm_b_gates"][0].reshape(16, 1)),
        "mlstm_conv_w": f(inp["mlstm_conv_w"][0]), "mlstm_conv_b": f(inp["mlstm_conv_b"][0].reshape(1, 2048)),
        "mlstm_norm_g": f(inp["mlstm_norm_g"][0].reshape(1, 2048)), "mlstm_w_out": f(inp["mlstm_w_out"][0]),
        "ln_mix_g": f(inp["ln_mix_g"]), "ln_mix_b": f(inp["ln_mix_b"]),
        "ln_ffn_g": f(inp["ln_ffn_g"]), "ln_ffn_b": f(inp["ln_ffn_b"]),
        "router_w": f(inp["router_w"]), "router_b": f(inp["router_b"].reshape(1, 16)),
        "moe_w_gate": f(inp["moe_w_gate"]), "moe_w_up": f(inp["moe_w_up"]), "moe_w_down": f(inp["moe_w_down"]),
    }
    shared.update(make_consts())
    x = np.asarray(inp["x"], dtype=np.float32)
    maps = []
    for c in range(ncores):
        m = dict(shared)
        m["x"] = np.ascontiguousarray(x[2 * c:2 * c + 2].reshape(NTOK, D))
        maps.append(m)
    return maps


def kernel(**inputs):
    nc = build()
    maps = make_in_maps(inputs)
    res = run_bass_kernel_spmd(nc, maps, core_ids=list(range(8)))
    out = np.stack([r["y"].reshape(2, 2048, D) for r in res.results], 0).reshape(16, 2048, D)
    return out.astype(np.float32)
```

```python
import numpy as np
from contextlib import ExitStack
import concourse.bass as bass
import concourse.mybir as mybir
from concourse.bass_utils import run_bass_kernel_spmd

F32 = mybir.dt.float32
BF16 = mybir.dt.bfloat16
AF = mybir.ActivationFunctionType
ALU = mybir.AluOpType
AX = mybir.AxisListType

NTOK = 4096
D = 2048
ALPHA = 4.0 ** 0.25
EPS_LN = 1e-5 / (ALPHA * ALPHA)
NEG = -1e30


class Tile:
    def __init__(self, t):
        self.t = t
        self.w = None
        self.r = {}

    def __getitem__(self, k):
        return self.t[k]


class Sched:
    def __init__(self, nc, st):
        self.nc = nc
        self.eng = {'pe': nc.tensor, 'act': nc.scalar, 'dve': nc.vector, 'pool': nc.gpsimd, 'sp': nc.sync}
        self.sem = {k: st.enter_context(nc.semaphore('s_' + k)) for k in self.eng}
        self.cnt = {k: 0 for k in self.eng}
        self.ND = 24
        self.dsem = [st.enter_context(nc.semaphore('d%d' % i)) for i in range(self.ND)]
        self.dcnt = [0] * self.ND
        self.dnext = 0
        self.NSW = 4
        self.swnext = 0
        self.known = {k: {} for k in self.eng}
        self.uid = 0

    def sb(self, st, shape, dt, name=None):
        self.uid += 1
        return Tile(st.enter_context(self.nc.sbuf_tensor('%s_%d' % (name or 'sb', self.uid), list(shape), dt)))

    def ps(self, st, shape, dt, name=None):
        self.uid += 1
        return Tile(st.enter_context(self.nc.psum_tensor('%s_%d' % (name or 'ps', self.uid), list(shape), dt)))

    def _semof(self, key):
        return self.sem[key] if isinstance(key, str) else self.dsem[key[1]]

    def _wait(self, eng, deps):
        kn = self.known[eng]
        for key, val in deps.items():
            if kn.get(key, 0) >= val:
                continue
            self.eng[eng].wait_ge(self._semof(key), val)
            kn[key] = val

    def _deps(self, eng, reads, writes):
        d = {}

        def add(k, v):
            if d.get(k, 0) < v:
                d[k] = v
        for t in reads:
            if t.w is not None:
                add(*t.w)
        for t in writes:
            if t.w is not None:
                if not (eng == 'pe' and t.w[0] == 'pe'):
                    add(*t.w)
            for k, v in t.r.items():
                add(k, v)
        return d

    def op(self, eng, fn, reads=(), writes=()):
        self._wait(eng, self._deps(eng, reads, writes))
        ins = fn(self.eng[eng])
        self.cnt[eng] += 1
        n = self.cnt[eng]
        ins.then_inc(self.sem[eng], 1)
        for t in reads:
            t.r[eng] = n
        for t in writes:
            t.w = (eng, n)
            t.r = {}

    def dma(self, out, in_, reads=(), writes=(), q='sp'):
        if q == 'pool':
            j = self.ND - self.NSW + self.swnext
            self.swnext = (self.swnext + 1) % self.NSW
        else:
            j = self.dnext
            self.dnext = (j + 1) % (self.ND - self.NSW)
        d = self._deps(q, reads, writes)
        if self.dcnt[j] > 0:
            k = ('d', j)
            d[k] = max(d.get(k, 0), self.dcnt[j])
        self._wait(q, d)
        self.dcnt[j] += 16
        self.eng[q].dma_start(out=out, in_=in_).then_inc(self.dsem[j], 16)
        k = ('d', j)
        for t in reads:
            t.r[k] = self.dcnt[j]
        for t in writes:
            t.w = (k, self.dcnt[j])
            t.r = {}

    def barrier(self):
        d = {k: v for k, v in self.cnt.items() if v > 0}
        for j in range(self.ND):
            if self.dcnt[j] > 0:
                d[('d', j)] = self.dcnt[j]
        for e in self.eng:
            self._wait(e, dict(d))


class Rot:
    def __init__(self, tiles):
        self.tiles = tiles
        self.i = 0

    def next(self):
        t = self.tiles[self.i % len(self.tiles)]
        self.i += 1
        return t


FULL = dict(nseq=2, nhead=8, nB=16, nln=32, ntt=8, nexp=16, ncc=16, nblk=16, nch=32, ncellh=8)


def build(upto=99, taps=False, cfg=None):
    C = dict(FULL)
    C.update(cfg or {})
    nc = bass.Bass("TRN2", target_bir_lowering=False)

    def din(name, shape, dt=F32):
        return nc.dram_tensor(name, list(shape), dt, kind="ExternalInput").ap()

    def dscr(name, shape, dt):
        return nc.dram_tensor(name, list(shape), dt, kind=("ExternalOutput" if taps else "Internal")).ap()

    x_in = din("x", [NTOK, D])
    w_qkv = din("attn_w_qkv", [D, 9216])
    w_o = din("attn_w_o", [1024, D])
    w_in = din("mlstm_w_in", [D, 6160])
    b_gates = din("mlstm_b_gates", [16, 1])
    conv_w = din("mlstm_conv_w", [4, 2048])
    conv_b = din("mlstm_conv_b", [1, 2048])
    norm_g = din("mlstm_norm_g", [1, 2048])
    w_out = din("mlstm_w_out", [D, D])
    ln_g = [din("ln_mix_g", [2, D]), din("ln_ffn_g", [2, D])]
    ln_b = [din("ln_mix_b", [2, D]), din("ln_ffn_b", [2, D])]
    router_w = din("router_w", [D, 16])
    router_b = din("router_b", [1, 16])
    NE = C.get("ne_decl", 16)
    moe_wg = din("moe_w_gate", [2, NE, D, 768])
    moe_wu = din("moe_w_up", [2, NE, D, 768])
    moe_wd = din("moe_w_down", [2, NE, 768, D])
    c_amask = din("c_amask", [128, 23 * 128])
    c_rope = din("c_rope", [32, 2, 2048])
    c_pm = din("c_pm", [32, 32])
    c_mt = din("c_mt", [64, 64])
    c_sel = din("c_sel", [8, 8, 128])
    y_out = nc.dram_tensor("y", [NTOK, D], F32, kind="ExternalOutput").ap()

    XT = dscr("XT", [D, NTOK], BF16)
    XRa = dscr("XRa", [NTOK, D], F32)
    XRb = dscr("XRb", [NTOK, D], F32)
    OT = dscr("OT", [D, NTOK], BF16)
    QKT = dscr("QKT", [D, NTOK], BF16)
    VV = dscr("VV", [NTOK, D], BF16)
    OG = dscr("OG", [NTOK, D], BF16)

    gst = ExitStack()
    S = Sched(nc, gst)

    ident = S.sb(gst, [128, 128], F32, 'ident')
    identb = S.sb(gst, [128, 128], BF16, 'identb')
    Gt = S.sb(gst, [128, 32, 16], F32, 'G')
    wr = S.sb(gst, [128, 16, 16], F32, 'wr')
    brb = S.sb(gst, [128, 16], F32, 'brb')
    mhalf = S.sb(gst, [128, 1], F32, 'mhalf')
    gbc = S.sb(gst, [128, D], F32, 'gbc')
    bbc = S.sb(gst, [128, D], F32, 'bbc')
    sm = S.sb(gst, [128, 256], F32, 'sm')

    S.op('pool', lambda e: e.memset(ident.t[:], 1.0), writes=[ident])
    S.op('pool', lambda e: e.affine_select(out=ident.t[:], in_=ident.t[:], pattern=[[-1, 128]],
                                           compare_op=ALU.is_equal, fill=0.0, base=0, channel_multiplier=1),
         reads=[ident], writes=[ident])
    S.op('pool', lambda e: e.tensor_copy(identb.t[:], ident.t[:]), reads=[ident], writes=[identb])
    S.op('pool', lambda e: e.memset(mhalf.t[:], -0.5), writes=[mhalf])
    S.dma(wr.t[:], router_w.rearrange("(c p) n -> p c n", p=128), writes=[wr])
    S.dma(brb.t[:], router_b.partition_broadcast(128), writes=[brb])
    wrh = S.sb(gst, [128, 16, 16], BF16, 'wrh')
    wrl = S.sb(gst, [128, 16, 16], BF16, 'wrl')
    S.op('dve', lambda e: e.tensor_copy(wrh.t[:], wr.t[:]), reads=[wr], writes=[wrh])
    S.op('dve', lambda e: e.tensor_tensor(wrl.t[:], wr.t[:], wrh.t[:], ALU.subtract), reads=[wr, wrh], writes=[wrl])

    def load_gb(which, layer):
        S.dma(gbc.t[:], ln_g[which][layer:layer + 1, :].partition_broadcast(128), writes=[gbc])
        S.dma(bbc.t[:], ln_b[which][layer:layer + 1, :].partition_broadcast(128), writes=[bbc])

    cast_rr = [0]

    def wload(stage_rot, dst_tile, dst_ap, src_ap, shape):
        stg = stage_rot.next()
        if len(shape) == 3:
            sap = stg.t[:, 0:shape[1] * shape[2]].rearrange("p (a b) -> p a b", a=shape[1])
        else:
            sap = stg.t[:, 0:shape[1]]
        S.dma(sap, src_ap, writes=[stg])
        eng = ('pool', 'pool', 'act')[cast_rr[0] % 3]
        cast_rr[0] += 1
        if eng == 'act':
            S.op('act', lambda e: e.copy(dst_ap, sap), reads=[stg], writes=[dst_tile])
        else:
            S.op(eng, lambda e: e.tensor_copy(dst_ap, sap), reads=[stg], writes=[dst_tile])

    def tstore(src, nblk, DST, row0, tok0, psl, xt, is_f32, xtf=None):
        per = 4 if is_f32 else 8
        idt = ident if is_f32 else identb
        ngrp = (nblk + per - 1) // per
        used = []
        for g in range(ngrp):
            p = psl[g % len(psl)]
            n = min(per, nblk - g * per)

            def f(e, g=g, p=p, n=n):
                for k in range(n):
                    c = g * per + k
                    ins = e.transpose(p.t[:, k * 128:(k + 1) * 128], src.t[:, c * 128:(c + 1) * 128], idt.t[:])
                return ins
            S.op('pe', f, reads=[src, idt], writes=[p])
            used.append((p, g, n))

        def ev(e):
            for (p, g, n) in used:
                ins = e.copy(xt.t[:, g * per:g * per + n, :],
                             p.t[:, 0:n * 128].rearrange("p (k t) -> p k t", k=n))
            return ins
        S.op('act', ev, reads=[u[0] for u in used], writes=[xt])
        if xtf is not None:
            xf = xtf[0]

            def ev2(e):
                for (p, g, n) in used:
                    ins = e.copy(xf.t[:, g * per:g * per + n, :],
                                 p.t[:, 0:n * 128].rearrange("p (k t) -> p k t", k=n))
                return ins
            S.op('act', ev2, reads=[u[0] for u in used], writes=[xf])
        S.dma(DST[row0:row0 + nblk * 128, tok0:tok0 + 128].rearrange("(c p) t -> p c t", p=128),
              xt.t[:, 0:nblk, :], reads=[xt])

    def ln_tile(t, ti, Xout, psl, xt, xtf=None, psr=None, do_xt=True):
        tok0 = ti * 128
        st6 = sm.t[:, 0:24].rearrange("p (a b) -> p a b", a=4)
        mv = sm.t[:, 24:26]
        rs = sm.t[:, 26:27]

        def f1(e):
            for k in range(4):
                ins = e.bn_stats(st6[:, k, :], t.t[:, k * 512:(k + 1) * 512])
            return ins
        S.op('dve', f1, reads=[t], writes=[sm])
        S.op('dve', lambda e: e.bn_aggr(mv, sm.t[:, 0:24]), reads=[sm], writes=[sm])
        S.op('pool', lambda e: e.tensor_scalar(rs, sm.t[:, 25:26], EPS_LN, None, ALU.add), reads=[sm], writes=[sm])
        S.op('pool', lambda e: e.tensor_tensor(rs, rs, mhalf.t[:], ALU.pow), reads=[sm, mhalf], writes=[sm])
        S.op('dve', lambda e: e.tensor_scalar(t.t[:], t.t[:], sm.t[:, 24:25], rs, ALU.subtract, ALU.mult),
             reads=[t, sm], writes=[t])
        S.op('dve', lambda e: e.tensor_tensor(t.t[:], t.t[:], gbc.t[:], ALU.mult), reads=[t, gbc], writes=[t])
        S.op('dve', lambda e: e.tensor_tensor(t.t[:], t.t[:], bbc.t[:], ALU.add), reads=[t, bbc], writes=[t])
        S.dma(Xout[tok0:tok0 + 128, :], t.t[:], reads=[t])
        if do_xt:
            tstore(t, 16, XT, 0, tok0, psl, xt, True, xtf)
        if xtf is not None:
            router(ti, xtf, psr, xt)

    def router(ti, xtf_, psr, xt):
        xtf, xl = xtf_
        rst = C.get('rstage', 9)
        if rst < 1:
            return
        S.op('dve', lambda e: e.tensor_tensor(xl.t[:], xtf.t[:], xt.t[:], ALU.subtract), reads=[xtf, xt], writes=[xl])

        def f(e):
            k = 0
            for c in range(16):
                for (a, b) in ((xt, wrh), (xt, wrl), (xl, wrh)):
                    ins = e.matmul(psr.t[:, 0:16], a.t[:, c, :], b.t[:, c, :], start=(k == 0), stop=(k == 47))
                    k += 1
            return ins
        if rst < 2:
            return
        S.op('pe', f, reads=[xt, xl, wrh, wrl], writes=[psr])
        if rst < 3:
            return
        lg = sm.t[:, 32:48]
        pr = sm.t[:, 48:64]
        pr3 = pr.rearrange("p (a b) -> p a b", a=4)
        eq1 = sm.t[:, 64:80]
        eq13 = eq1.rearrange("p (a b) -> p a b", a=4)
        pr2 = sm.t[:, 80:96]
        pr23 = pr2.rearrange("p (a b) -> p a b", a=4)
        eq2 = sm.t[:, 96:112]
        eq23 = eq2.rearrange("p (a b) -> p a b", a=4)
        m1 = sm.t[:, 112:116]
        m2 = sm.t[:, 116:120]
        sc = sm.t[:, 120:124]
        geq = sm.t[:, 124:128]
        mx = sm.t[:, 128:129]
        ssum = sm.t[:, 129:130]
        gm = sm.t[:, 130:131]
        den = sm.t[:, 131:132]
        def G4(a, g):
            return a[:, 4 * g:4 * g + 4]
        seq = [
            ('dve', lambda e: e.tensor_tensor(lg, psr.t[:, 0:16], brb.t[:], ALU.add)),
            ('dve', lambda e: e.reduce_max(mx, lg, AX.X)),
            ('dve', lambda e: e.tensor_scalar(mx, mx, -1.0, None, ALU.mult)),
            ('act', lambda e: e.activation(pr, lg, AF.Exp, bias=mx, scale=1.0)),
            ('dve', lambda e: e.reduce_sum(ssum, pr, AX.X)),
            ('dve', lambda e: e.reciprocal(ssum, ssum)),
            ('dve', lambda e: e.tensor_scalar(pr, pr, ssum, None, ALU.mult)),
        ]
        for g in range(4):
            seq.append(('dve', lambda e, g=g: e.reduce_max(m1[:, g:g + 1], G4(pr, g), AX.X)))
        for g in range(4):
            seq.append(('dve', lambda e, g=g: e.tensor_scalar(G4(eq1, g), G4(pr, g), m1[:, g:g + 1], None, ALU.is_ge)))
        seq.append(('dve', lambda e: e.scalar_tensor_tensor(pr2, eq1, -2.0, pr, ALU.mult, ALU.add)))
        for g in range(4):
            seq.append(('dve', lambda e, g=g: e.reduce_max(m2[:, g:g + 1], G4(pr2, g), AX.X)))
        for g in range(4):
            seq.append(('dve', lambda e, g=g: e.tensor_scalar(G4(eq2, g), G4(pr2, g), m2[:, g:g + 1], None, ALU.is_ge)))
        seq += [
            ('dve', lambda e: e.tensor_tensor(sc, m1, m2, ALU.add)),
            ('dve', lambda e: e.reduce_max(gm, sc, AX.X)),
            ('dve', lambda e: e.tensor_scalar(geq, sc, gm, None, ALU.is_ge)),
            ('dve', lambda e: e.tensor_tensor(eq1, eq1, eq2, ALU.add)),
        ]
        for g in range(4):
            seq.append(('dve', lambda e, g=g: e.tensor_scalar(G4(eq1, g), G4(eq1, g), geq[:, g:g + 1], None, ALU.mult)))
        seq += [
            ('dve', lambda e: e.tensor_tensor(pr, pr, eq1, ALU.mult)),
            ('dve', lambda e: e.reduce_sum(den, pr, AX.X)),
            ('dve', lambda e: e.reciprocal(den, den)),
            ('dve', lambda e: e.tensor_scalar(Gt.t[:, ti, :], pr, den, 1.0 / ALPHA, ALU.mult, ALU.mult)),
        ]
        for i, (eng, fn) in enumerate(seq):
            rd = [sm, psr, brb] if i == 0 else [sm]
            wt = [sm, Gt] if i == len(seq) - 1 else [sm]
            S.op(eng, fn, reads=rd, writes=wt)

    def phase_x2xt():
        with ExitStack() as st:
            xs = Rot([S.sb(st, [128, D], F32) for _ in range(2)])
            xts = Rot([S.sb(st, [128, 16, 128], BF16) for _ in range(2)])
            psl = [S.ps(st, [128, 512], F32) for _ in range(4)]
            for i in range(32):
                x = xs.next()
                S.dma(x.t[:], x_in[i * 128:(i + 1) * 128, :], writes=[x])
                tstore(x, 16, XT, 0, i * 128, psl, xts.next(), True)
            S.barrier()

    GD = [1, 4, 15]
    MOFF = [0, 2, 7]

    def phase_attn():
        with ExitStack() as st:
            stage = Rot([S.sb(st, [128, 2048], F32) for _ in range(1)])
            wq = [S.sb(st, [128, 16, 128], BF16) for _ in range(9)]
            xtl = Rot([S.sb(st, [128, 16, 512], BF16) for _ in range(1)])
            QK = [[S.sb(st, [128, 2048], BF16) for _ in range(2)] for _ in range(3)]
            Vt = [S.sb(st, [128, 16, 128], BF16) for _ in range(3)]
            amask = S.sb(st, [128, 23 * 128], F32)
            rope = S.sb(st, [32, 2, 2048], F32)
            pmf = S.sb(st, [32, 32], F32)
            pm = S.sb(st, [32, 32], BF16)
            SsbR = [S.sb(st, [128, 23 * 128], F32) for _ in range(2)]
            PbR = [S.sb(st, [128, 23 * 128], BF16) for _ in range(2)]
            PTR = [S.sb(st, [128, 23 * 128], BF16) for _ in range(2)]
            smsR = [S.sb(st, [128, 8], F32) for _ in range(2)]
            osb = Rot([S.sb(st, [128, 128], BF16) for _ in range(2)])
            r1 = S.sb(st, [32, 512], F32)
            r2 = S.sb(st, [32, 512], F32)
            ots = Rot([S.sb(st, [128, 128], BF16) for _ in range(2)])
            psA = Rot([S.ps(st, [128, 512], F32) for _ in range(3)])
            psT = Rot([S.ps(st, [128, 1024], BF16) for _ in range(3)])
            psO = Rot([S.ps(st, [128, 512], F32) for _ in range(2)])
            S.dma(amask.t[:], c_amask, writes=[amask])
            S.dma(rope.t[:], c_rope, writes=[rope])
            S.dma(pmf.t[:], c_pm, writes=[pmf])
            S.op('pool', lambda e: e.tensor_copy(pm.t[:], pmf.t[:]), reads=[pmf], writes=[pm])
            scale = 128.0 ** -0.5
            for s in range(C['nseq']):
                for h in range(C['nhead']):
                    for g in range(3):
                        for j in range(3):
                            wt = wq[g * 3 + j]
                            col = g * 3072 + j * 1024 + h * 128
                            wload(stage, wt, wt.t[:], w_qkv[:, col:col + 128].rearrange("(c p) n -> p c n", p=128),
                                  [128, 16, 128])
                    for tt in range(4):
                        xt_ = xtl.next()
                        S.dma(xt_.t[:], XT[:, s * 2048 + tt * 512: s * 2048 + (tt + 1) * 512]
                              .rearrange("(c p) t -> p c t", p=128), writes=[xt_])
                        cs = slice(tt * 512, (tt + 1) * 512)
                        for g in range(3):
                            for j in range(3):
                                wt = wq[g * 3 + j]
                                p = psA.next()
                                if j < 2:
                                    def f(e, p=p, wt=wt, xt_=xt_):
                                        for c in range(16):
                                            ins = e.matmul(p.t[:], wt.t[:, c, :], xt_.t[:, c, :],
                                                           start=(c == 0), stop=(c == 15))
                                        return ins
                                    S.op('pe', f, reads=[wt, xt_], writes=[p])
                                    dst = QK[g][j]
                                    S.op('act', lambda e, p=p, dst=dst, j=j: e.activation(
                                        dst.t[:, cs], p.t[:], AF.Copy, scale=(scale if j == 0 else 1.0)),
                                        reads=[p], writes=[dst])
                                    p2 = psO.next()
                                    S.op('pe', lambda e, p2=p2, dst=dst: e.matmul(
                                        p2.t[0:32, :], pm.t[:], dst.t[0:32, cs], start=True, stop=True),
                                        reads=[pm, dst], writes=[p2])
                                    S.op('dve', lambda e, dst=dst: e.tensor_tensor(
                                        r1.t[:], dst.t[0:32, cs], rope.t[:, 0, cs], ALU.mult),
                                        reads=[dst, rope], writes=[r1])
                                    S.op('dve', lambda e, p2=p2: e.tensor_tensor(
                                        r2.t[:], p2.t[0:32, :], rope.t[:, 1, cs], ALU.mult),
                                        reads=[p2, rope], writes=[r2])
                                    S.op('dve', lambda e, dst=dst: e.tensor_tensor(
                                        dst.t[0:32, cs], r1.t[:], r2.t[:], ALU.add),
                                        reads=[r1, r2], writes=[dst])
                                else:
                                    def f(e, p=p, wt=wt, xt_=xt_):
                                        for b in range(4):
                                            for c in range(16):
                                                ins = e.matmul(p.t[:, b * 128:(b + 1) * 128],
                                                               xt_.t[:, c, b * 128:(b + 1) * 128], wt.t[:, c, :],
                                                               start=(c == 0), stop=(c == 15))
                                        return ins
                                    S.op('pe', f, reads=[wt, xt_], writes=[p])
                                    S.op('act', lambda e, p=p, g=g: e.copy(
                                        Vt[g].t[:, tt * 4:(tt + 1) * 4, :],
                                        p.t[:].rearrange("p (b n) -> p b n", b=4)), reads=[p], writes=[Vt[g]])
                    def stage1(B):
                        Ssb = SsbR[B % 2]
                        col = 0
                        for g in range(3):
                            jb = max(0, B - GD[g])
                            while jb <= B:
                                n = min(4, B + 1 - jb)
                                p = psA.next()
                                S.op('pe', lambda e, p=p, g=g, jb=jb, n=n: e.matmul(
                                    p.t[:, 0:n * 128], QK[g][0].t[:, B * 128:(B + 1) * 128],
                                    QK[g][1].t[:, jb * 128:(jb + n) * 128], start=True, stop=True),
                                    reads=[QK[g][0], QK[g][1]], writes=[p])
                                mo = (MOFF[g] + GD[g] - B + jb) * 128
                                S.op('dve', lambda e, p=p, n=n, col=col, mo=mo: e.tensor_tensor(
                                    Ssb.t[:, col:col + n * 128], p.t[:, 0:n * 128], amask.t[:, mo:mo + n * 128], ALU.add),
                                    reads=[p, amask], writes=[Ssb])
                                col += n * 128
                                jb += n

                    def stage2(B):
                        Ssb, Pb, PT, sms = SsbR[B % 2], PbR[B % 2], PTR[B % 2], smsR[B % 2]
                        kb = []
                        for g in range(3):
                            for jb in range(max(0, B - GD[g]), B + 1):
                                kb.append((g, jb))
                        nb = len(kb)
                        nc_ = nb * 128
                        S.op('dve', lambda e: e.reduce_max(sms.t[:, 0:1], Ssb.t[:, 0:nc_], AX.X),
                             reads=[Ssb], writes=[sms])
                        S.op('dve', lambda e: e.tensor_scalar(sms.t[:, 1:2], sms.t[:, 0:1], -1.0, None, ALU.mult),
                             reads=[sms], writes=[sms])
                        S.op('act', lambda e: e.activation(Pb.t[:, 0:nc_], Ssb.t[:, 0:nc_], AF.Exp,
                                                           bias=sms.t[:, 1:2], scale=1.0, accum_out=sms.t[:, 2:3]),
                             reads=[Ssb, sms], writes=[Pb, sms])
                        S.op('dve', lambda e: e.reciprocal(sms.t[:, 3:4], sms.t[:, 2:3]), reads=[sms], writes=[sms])
                        i = 0
                        while i < nb:
                            n = min(8, nb - i)
                            pt = psT.next()

                            def f(e, pt=pt, i=i, n=n):
                                for k in range(n):
                                    ins = e.transpose(pt.t[:, k * 128:(k + 1) * 128],
                                                      Pb.t[:, (i + k) * 128:(i + k + 1) * 128], identb.t[:])
                                return ins
                            S.op('pe', f, reads=[Pb, identb], writes=[pt])
                            if (i // 8) % 2 == 0:
                                S.op('act', lambda e, pt=pt, i=i, n=n: e.copy(
                                    PT.t[:, i * 128:(i + n) * 128], pt.t[:, 0:n * 128]), reads=[pt], writes=[PT])
                            else:
                                S.op('dve', lambda e, pt=pt, i=i, n=n: e.tensor_copy(
                                    PT.t[:, i * 128:(i + n) * 128], pt.t[:, 0:n * 128]), reads=[pt], writes=[PT])
                            i += n
                        po = psO.next()

                        def f(e, po=po, kb=kb, nb=nb):
                            for i, (g, jb) in enumerate(kb):
                                ins = e.matmul(po.t[:, 0:128], PT.t[:, i * 128:(i + 1) * 128], Vt[g].t[:, jb, :],
                                               start=(i == 0), stop=(i == nb - 1))
                            return ins
                        S.op('pe', f, reads=[PT] + Vt, writes=[po])
                        ob = osb.next()
                        S.op('act', lambda e, po=po, ob=ob: e.activation(
                            ob.t[:], po.t[:, 0:128], AF.Copy, scale=sms.t[:, 3:4]),
                            reads=[po, sms], writes=[ob])
                        pt = psT.next()
                        S.op('pe', lambda e, pt=pt, ob=ob: e.transpose(pt.t[:, 0:128], ob.t[:], identb.t[:]),
                             reads=[ob, identb], writes=[pt])
                        ot = ots.next()
                        S.op('act', lambda e, pt=pt, ot=ot: e.copy(ot.t[:], pt.t[:, 0:128]), reads=[pt], writes=[ot])
                        t0 = s * 2048 + B * 128
                        S.dma(OT[h * 128:(h + 1) * 128, t0:t0 + 128], ot.t[:], reads=[ot])
                    nB = C['nB']
                    stage1(0)
                    for B in range(nB):
                        if B + 1 < nB:
                            stage1(B + 1)
                        stage2(B)
            S.barrier()

    def tstore_view(tile, view, nblk, DST, row0, tok0, psl, xt):
        p = psl[(tok0 // 128) % len(psl)]

        def f(e):
            for k in range(nblk):
                ins = e.transpose(p.t[:, k * 128:(k + 1) * 128], view[:, k * 128:(k + 1) * 128], identb.t[:])
            return ins
        S.op('pe', f, reads=[tile, identb], writes=[p])
        S.op('act', lambda e: e.copy(xt.t[:, 0:nblk, :], p.t[:, 0:nblk * 128].rearrange("p (k t) -> p k t", k=nblk)),
             reads=[p], writes=[xt])
        S.dma(DST[row0:row0 + nblk * 128, tok0:tok0 + 128].rearrange("(c p) t -> p c t", p=128),
              xt.t[:, 0:nblk, :], reads=[xt])

    def phase_proj_ln(KC, Wd, INT, resid, which, layer, Xout, with_router):
        with ExitStack() as st:
            stage = Rot([S.sb(st, [128, 2048], F32) for _ in range(2)])
            W = S.sb(st, [128, KC, D], BF16)
            ins_ = Rot([S.sb(st, [128, KC, 128], BF16) for _ in range(2)])
            rt = Rot([S.sb(st, [128, D], F32) for _ in range(2)])
            xts = Rot([S.sb(st, [128, 16, 128], BF16) for _ in range(2)])
            xtf = (S.sb(st, [128, 16, 128], F32), S.sb(st, [128, 16, 128], BF16)) if with_router else None
            psl = [S.ps(st, [128, 512], F32) for _ in range(4)]
            psm = [S.ps(st, [128, 512], F32) for _ in range(3)]
            psr = S.ps(st, [128, 512], F32) if with_router else None
            load_gb(which, layer)
            for c in range(KC):
                wload(stage, W, W.t[:, c, :], Wd[c * 128:(c + 1) * 128, :], [128, D])
            for ti in range(C['nln']):
                it = ins_.next()
                S.dma(it.t[:], INT[0:KC * 128, ti * 128:(ti + 1) * 128].rearrange("(c p) t -> p c t", p=128),
                      writes=[it])
                t = rt.next()
                S.dma(t.t[:], resid[ti * 128:(ti + 1) * 128, :], writes=[t])
                for dc in range(4):
                    p = psm[dc % 3]

                    def f(e, p=p, dc=dc, it=it):
                        for c in range(KC):
                            ins = e.matmul(p.t[:], it.t[:, c, :], W.t[:, c, dc * 512:(dc + 1) * 512],
                                           start=(c == 0), stop=(c == KC - 1))
                        return ins
                    S.op('pe', f, reads=[it, W], writes=[p])
                    S.op('dve', lambda e, p=p, dc=dc, t=t: e.scalar_tensor_tensor(
                        t.t[:, dc * 512:(dc + 1) * 512], p.t[:], 1.0 / ALPHA, t.t[:, dc * 512:(dc + 1) * 512],
                        ALU.mult, ALU.add), reads=[p, t], writes=[t])
                ln_tile(t, ti, Xout, psl, xts.next(), xtf, psr)
            S.barrier()

    def phase_moe(layer, resid, Xout, final):
        with ExitStack() as st:
            stage = Rot([S.sb(st, [128, 2048], F32) for _ in range(3)])
            wgu = Rot([S.sb(st, [128, 16, 128], BF16) for _ in range(6)])
            wdl = Rot([S.sb(st, [128, 6, D], BF16) for _ in range(2)])
            xt_ = S.sb(st, [128, 16, 512], BF16)
            Y = [S.sb(st, [128, D], F32) for _ in range(4)]
            hT = [S.sb(st, [128, 512], BF16) for _ in range(6)]
            sg = Rot([S.sb(st, [128, 512], F32) for _ in range(2)])
            xts = Rot([S.sb(st, [128, 16, 128], BF16) for _ in range(2)])
            psG = Rot([S.ps(st, [128, 512], F32) for _ in range(2)])
            psU = Rot([S.ps(st, [128, 512], F32) for _ in range(2)])
            psD = Rot([S.ps(st, [128, 512], F32) for _ in range(4)])
            load_gb(1, layer)
            for tt in range(C['ntt']):
                S.dma(xt_.t[:], XT[:, tt * 512:(tt + 1) * 512].rearrange("(c p) t -> p c t", p=128), writes=[xt_])
                for b in range(4):
                    S.dma(Y[b].t[:], resid[tt * 512 + b * 128: tt * 512 + (b + 1) * 128, :], writes=[Y[b]])
                for e_ in range(C['nexp']):
                    wd = wdl.next()
                    S.dma(wd.t[:], moe_wd[layer, e_].rearrange("(c p) n -> p c n", p=128), writes=[wd], q='pool')
                    for fc in range(6):
                        wg = wgu.next()
                        wu = wgu.next()
                        wload(stage, wg, wg.t[:], moe_wg[layer, e_, :, fc * 128:(fc + 1) * 128]
                              .rearrange("(c p) n -> p c n", p=128), [128, 16, 128])
                        wload(stage, wu, wu.t[:], moe_wu[layer, e_, :, fc * 128:(fc + 1) * 128]
                              .rearrange("(c p) n -> p c n", p=128), [128, 16, 128])
                        pg = psG.next()
                        pu = psU.next()

                        def f(e, pg=pg, wg=wg):
                            for c in range(16):
                                ins = e.matmul(pg.t[:], wg.t[:, c, :], xt_.t[:, c, :], start=(c == 0), stop=(c == 15))
                            return ins
                        S.op('pe', f, reads=[wg, xt_], writes=[pg])

                        def f(e, pu=pu, wu=wu):
                            for c in range(16):
                                ins = e.matmul(pu.t[:], wu.t[:, c, :], xt_.t[:, c, :], start=(c == 0), stop=(c == 15))
                            return ins
                        S.op('pe', f, reads=[wu, xt_], writes=[pu])
                        sg_ = sg.next()
                        S.op('act', lambda e, pg=pg, sg_=sg_: e.activation(sg_.t[:], pg.t[:], AF.Silu),
                             reads=[pg], writes=[sg_])
                        S.op('dve', lambda e, pu=pu, sg_=sg_, fc=fc: e.tensor_tensor(
                            hT[fc].t[:], sg_.t[:], pu.t[:], ALU.mult), reads=[sg_, pu], writes=[hT[fc]])
                    for b in range(4):
                        for dc in range(4):
                            pd = psD.next()

                            def f(e, pd=pd, b=b, dc=dc, wd=wd):
                                for fc in range(6):
                                    ins = e.matmul(pd.t[:], hT[fc].t[:, b * 128:(b + 1) * 128],
                                                   wd.t[:, fc, dc * 512:(dc + 1) * 512],
                                                   start=(fc == 0), stop=(fc == 5))
                                return ins
                            S.op('pe', f, reads=hT + [wd], writes=[pd])
                            gi = tt * 4 + b
                            S.op('dve', lambda e, pd=pd, b=b, dc=dc, gi=gi, e_=e_: e.scalar_tensor_tensor(
                                Y[b].t[:, dc * 512:(dc + 1) * 512], pd.t[:], Gt.t[:, gi, e_:e_ + 1],
                                Y[b].t[:, dc * 512:(dc + 1) * 512], ALU.mult, ALU.add),
                                reads=[pd, Gt, Y[b]], writes=[Y[b]])
                for b in range(4):
                    ln_tile(Y[b], tt * 4 + b, Xout, psD.tiles, xts.next(), do_xt=(not final))
            S.barrier()

    GS = dscr("GS", [16, NTOK], F32)

    def phase_mlstm_proj():
        with ExitStack() as st:
            stage = Rot([S.sb(st, [128, 2048], F32) for _ in range(2)])
            xs = S.sb(st, [128, 16, 2048], BF16)
            zTR = Rot([S.sb(st, [128, 2051], F32) for _ in range(2)])
            accR = Rot([S.sb(st, [128, 2048], F32) for _ in range(2)])
            qkb = Rot([S.sb(st, [128, 2048], BF16) for _ in range(2)])
            wsm = Rot([S.sb(st, [128, 16, 128], BF16) for _ in range(2)])
            wbg = Rot([S.sb(st, [128, 16, 512], BF16) for _ in range(2)])
            wgt = S.sb(st, [128, 16, 16], BF16)
            vb = Rot([S.sb(st, [128, 512], BF16) for _ in range(3)])
            gsb = Rot([S.sb(st, [8, 512], F32) for _ in range(2)])
            cw = S.sb(st, [128, 4, 16], F32)
            cb = S.sb(st, [128, 16], F32)
            psA = Rot([S.ps(st, [128, 512], F32) for _ in range(4)])
            with nc.allow_non_contiguous_dma(reason="tiny conv weight transpose"):
                for j in range(4):
                    S.dma(cw.t[:, j, :], conv_w[j:j + 1, :].rearrange("o (c p) -> p (o c)", p=128), writes=[cw])
                S.dma(cb.t[:], conv_b.rearrange("o (c p) -> p (o c)", p=128), writes=[cb])
            for zT in zTR.tiles:
                S.op('pool', lambda e, zT=zT: e.memset(zT.t[:, 0:3], 0.0), writes=[zT])
            wload(stage, wgt, wgt.t[:], w_in[:, 6144:6160].rearrange("(c p) n -> p c n", p=128), [128, 16, 16])
            for s in range(C['nseq']):
                S.dma(xs.t[:], XT[:, s * 2048:(s + 1) * 2048].rearrange("(c p) t -> p c t", p=128), writes=[xs])
                for tt in range(4):
                    for half in range(2):
                        p = psA.next()

                        def f(e, p=p, half=half, tt=tt):
                            for c in range(16):
                                ins = e.matmul(p.t[0:8, :], wgt.t[:, c, half * 8:(half + 1) * 8],
                                               xs.t[:, c, tt * 512:(tt + 1) * 512], start=(c == 0), stop=(c == 15))
                            return ins
                        S.op('pe', f, reads=[wgt, xs], writes=[p])
                        gt_ = gsb.next()
                        S.op('act', lambda e, p=p, gt_=gt_: e.copy(gt_.t[:], p.t[0:8, :]), reads=[p], writes=[gt_])
                        S.dma(GS[half * 8:(half + 1) * 8, s * 2048 + tt * 512: s * 2048 + (tt + 1) * 512], gt_.t[:],
                              reads=[gt_])
                for cc in (range(16) if C['ncc'] == 16 else (0, 8)):
                    w = wsm.next()
                    zT = zTR.next()
                    acc = accR.next()
                    wload(stage, w, w.t[:], w_in[:, cc * 128:(cc + 1) * 128].rearrange("(c p) n -> p c n", p=128),
                          [128, 16, 128])
                    for tt in range(4):
                        p = psA.next()

                        def f(e, p=p, w=w, tt=tt):
                            for c in range(16):
                                ins = e.matmul(p.t[:], w.t[:, c, :], xs.t[:, c, tt * 512:(tt + 1) * 512],
                                               start=(c == 0), stop=(c == 15))
                            return ins
                        S.op('pe', f, reads=[w, xs], writes=[p])
                        S.op('act', lambda e, p=p, tt=tt, zT=zT: e.copy(zT.t[:, 3 + tt * 512: 3 + (tt + 1) * 512], p.t[:]),
                             reads=[p], writes=[zT])
                    S.op('dve', lambda e, cc=cc, zT=zT, acc=acc: e.tensor_scalar(acc.t[:], zT.t[:, 3:2051], cw.t[:, 3, cc:cc + 1],
                                                                cb.t[:, cc:cc + 1], ALU.mult, ALU.add),
                         reads=[zT, cw, cb], writes=[acc])
                    for j in (2, 1, 0):
                        S.op('dve', lambda e, cc=cc, j=j, zT=zT, acc=acc: e.scalar_tensor_tensor(
                            acc.t[:], zT.t[:, j:j + 2048], cw.t[:, j, cc:cc + 1], acc.t[:], ALU.mult, ALU.add),
                            reads=[zT, cw, acc], writes=[acc])
                    S.op('act', lambda e, acc=acc: e.activation(acc.t[:], acc.t[:], AF.Silu), reads=[acc], writes=[acc])
                    qb = qkb.next()
                    S.op('pool', lambda e, qb=qb, cc=cc, acc=acc: e.tensor_scalar(
                        qb.t[:], acc.t[:], (1.0 if cc < 8 else 128.0 ** -0.5), None, ALU.mult),
                        reads=[acc], writes=[qb])
                    S.dma(QKT[cc * 128:(cc + 1) * 128, s * 2048:(s + 1) * 2048], qb.t[:], reads=[qb])
                for nch in range(8):
                    w = wbg.next()
                    c0 = 2048 + nch * 512
                    for c4 in range(4):
                        wload(stage, w, w.t[:, c4 * 4:(c4 + 1) * 4, :],
                              w_in[c4 * 512:(c4 + 1) * 512, c0:c0 + 512].rearrange("(c p) n -> p c n", p=128),
                              [128, 4, 512])
                    for blk in range(C['nblk']):
                        p = psA.next()

                        def f(e, p=p, w=w, blk=blk):
                            for c in range(16):
                                ins = e.matmul(p.t[:], xs.t[:, c, blk * 128:(blk + 1) * 128], w.t[:, c, :],
                                               start=(c == 0), stop=(c == 15))
                            return ins
                        S.op('pe', f, reads=[w, xs], writes=[p])
                        v_ = vb.next()
                        r0 = s * 2048 + blk * 128
                        if nch < 4:
                            S.op('act', lambda e, p=p, v_=v_: e.copy(v_.t[:], p.t[:]), reads=[p], writes=[v_])
                            S.dma(VV[r0:r0 + 128, nch * 512:(nch + 1) * 512], v_.t[:], reads=[v_])
                        else:
                            S.op('act', lambda e, p=p, v_=v_: e.activation(v_.t[:], p.t[:], AF.Sigmoid),
                                 reads=[p], writes=[v_])
                            S.dma(OG[r0:r0 + 128, (nch - 4) * 512:(nch - 3) * 512], v_.t[:], reads=[v_])
            S.barrier()

    def phase_mlstm_cell():
        with ExitStack() as st:
            qk = S.sb(st, [128, 16, 2048], BF16)
            cols = S.sb(st, [128, 1024], F32)
            decb = S.sb(st, [128, 256], F32)
            selt = S.sb(st, [8, 8, 128], F32)
            m01 = S.sb(st, [64, 64], F32)
            bg = S.sb(st, [8, 2], F32)
            vx = Rot([S.sb(st, [64, 8, 257], BF16) for _ in range(2)])
            ogt = Rot([S.sb(st, [64, 2048], BF16) for _ in range(2)])
            Yt = Rot([S.sb(st, [64, 2048], BF16) for _ in range(2)])
            yT = Rot([S.sb(st, [128, 16, 64], BF16) for _ in range(2)])
            Cx = [S.sb(st, [128, 257], F32) for _ in range(8)]
            Cb = [S.sb(st, [128, 257], BF16) for _ in range(8)]
            aT = Rot([S.sb(st, [64, 64], BF16) for _ in range(3)])
            vp = Rot([S.sb(st, [64, 257], BF16) for _ in range(3)])
            p1s = Rot([S.sb(st, [64, 257], F32) for _ in range(3)])
            num = Rot([S.sb(st, [64, 257], F32) for _ in range(3)])
            hn = Rot([S.sb(st, [64, 256], F32) for _ in range(3)])
            sst = Rot([S.sb(st, [64, 16], F32) for _ in range(3)])
            kcs = Rot([S.sb(st, [64, 128], BF16) for _ in range(3)])
            psS = Rot([S.ps(st, [128, 512], F32) for _ in range(2)])
            psP = Rot([S.ps(st, [128, 512], F32) for _ in range(2)])
            psU = Rot([S.ps(st, [128, 512], F32) for _ in range(2)])
            psB = Rot([S.ps(st, [128, 1024], BF16) for _ in range(2)])
            S.dma(selt.t[:], c_sel, writes=[selt])
            S.dma(m01.t[:], c_mt, writes=[m01])
            S.op('dve', lambda e: e.tensor_scalar(m01.t[:], m01.t[:], 0.0, None, ALU.is_equal), reads=[m01], writes=[m01])
            S.dma(bg.t[:, 0:1], b_gates[0:8, :], writes=[bg])
            S.dma(bg.t[:, 1:2], b_gates[8:16, :], writes=[bg])
            S.dma(gbc.t[:], norm_g.partition_broadcast(128), writes=[gbc])
            for v_ in vx.tiles:
                S.op('pool', lambda e, v_=v_: e.memset(v_.t[:], 1.0), writes=[v_])
            for s in range(C['nseq']):
                S.dma(qk.t[:], QKT[:, s * 2048:(s + 1) * 2048].rearrange("(c p) t -> p c t", p=128), writes=[qk])
                with ExitStack() as st2:
                    def g8():
                        return S.sb(st2, [8, 2048], F32)
                    gi, gf, ta, tb, Bc, Wc, wv = g8(), g8(), g8(), g8(), g8(), g8(), g8()
                    small = S.sb(st2, [8, 256], F32)
                    mst = small.t[:, 0:33]
                    mul = small.t[:, 40:72]
                    dec = small.t[:, 80:112]
                    S.dma(gi.t[:], GS[0:8, s * 2048:(s + 1) * 2048], writes=[gi])
                    S.dma(gf.t[:], GS[8:16, s * 2048:(s + 1) * 2048], writes=[gf])

                    def v3(t_, sl=None):
                        a = t_.t[:, :].rearrange("p (c l) -> p c l", l=64)
                        return a

                    def D_(fn, rd, wt):
                        S.op('dve', fn, reads=rd, writes=wt)

                    def A_(fn, rd, wt):
                        S.op('act', fn, reads=rd, writes=wt)
                    D_(lambda e: e.tensor_scalar(gi.t[:], gi.t[:], bg.t[:, 0:1], None, ALU.add), [gi, bg], [gi])
                    D_(lambda e: e.tensor_scalar(gf.t[:], gf.t[:], bg.t[:, 1:2], None, ALU.add), [gf, bg], [gf])
                    D_(lambda e: e.scalar_tensor_tensor(ta.t[:], gf.t[:], -1.0, gf.t[:], ALU.mult, ALU.max), [gf], [ta])
                    A_(lambda e: e.activation(ta.t[:], ta.t[:], AF.Exp, scale=-1.0), [ta], [ta])
                    A_(lambda e: e.activation(ta.t[:], ta.t[:], AF.Ln, bias=1.0), [ta], [ta])
                    D_(lambda e: e.tensor_scalar(tb.t[:], gf.t[:], 0.0, None, ALU.min), [gf], [tb])
                    D_(lambda e: e.tensor_tensor(tb.t[:], tb.t[:], ta.t[:], ALU.subtract), [tb, ta], [tb])

                    def scan(src, dst, tmp, op):
                        cur, nxt = src, dst
                        for k in (1, 2, 4, 8, 16, 32):
                            c3, n3 = v3(cur), v3(nxt)
                            D_(lambda e, c3=c3, n3=n3, k=k: e.tensor_tensor(n3[:, :, k:], c3[:, :, k:], c3[:, :, :64 - k], op),
                               [cur], [nxt])
                            S.op('pool', lambda e, c3=c3, n3=n3, k=k: e.tensor_copy(n3[:, :, :k], c3[:, :, :k]), reads=[cur], writes=[nxt])
                            cur, nxt = nxt, (tmp if nxt is dst else dst)
                        return cur
                    Bres = scan(tb, Bc, ta, ALU.add)
                    D_(lambda e: e.tensor_tensor(wv.t[:], gi.t[:], Bres.t[:], ALU.subtract), [gi, Bres], [wv])
                    free = [t_ for t_ in (ta, tb, Bc) if t_ is not Bres]
                    Wres = scan(wv, free[0], free[1], ALU.max)
                    B3, W3 = v3(Bres), v3(Wres)
                    D_(lambda e: e.memset(small.t[:, 0:1], NEG), [], [small])
                    for c in range(32):
                        D_(lambda e, c=c: e.tensor_tensor(small.t[:, 40 + c:41 + c], small.t[:, c:c + 1],
                                                          W3[:, c, 63:64], ALU.max), [small, Wres], [small])
                        D_(lambda e, c=c: e.tensor_tensor(small.t[:, c + 1:c + 2], B3[:, c, 63:64],
                                                          small.t[:, 40 + c:41 + c], ALU.add), [small, Bres], [small])
                    D_(lambda e: e.tensor_tensor(dec, small.t[:, 0:32], mul, ALU.subtract), [small], [small])
                    A_(lambda e: e.activation(dec, dec, AF.Exp), [small], [small])
                    mb = small.t[:, 0:32].unsqueeze(2).broadcast_to([8, 32, 64])
                    mulb = mul.unsqueeze(2).broadcast_to([8, 32, 64])
                    D_(lambda e: e.tensor_tensor(W3, W3, mb, ALU.max), [Wres, small], [Wres])
                    mu = Wres
                    q_wk, q_rf, q_wi, q_th = gi, gf, free[0] if free[0] is not Wres else free[1], None
                    others = [t_ for t_ in (ta, tb, Bc) if t_ is not Bres and t_ is not Wres]
                    q_wi = others[0]
                    D_(lambda e: e.tensor_tensor(v3(q_wk), v3(wv), mulb, ALU.subtract), [wv, small], [q_wk])
                    A_(lambda e: e.activation(q_wk.t[:], q_wk.t[:], AF.Exp), [q_wk], [q_wk])
                    D_(lambda e: e.tensor_tensor(v3(q_rf), mulb, v3(mu), ALU.subtract), [mu, small], [q_rf])
                    A_(lambda e: e.activation(q_rf.t[:], q_rf.t[:], AF.Exp), [q_rf], [q_rf])
                    D_(lambda e: e.tensor_tensor(v3(q_wi), mb, v3(mu), ALU.subtract), [mu, small], [q_wi])
                    A_(lambda e: e.activation(q_wi.t[:], q_wi.t[:], AF.Exp), [q_wi], [q_wi])
                    D_(lambda e: e.tensor_tensor(wv.t[:], Bres.t[:], mu.t[:], ALU.add), [Bres, mu, wv], [wv])
                    A_(lambda e: e.activation(wv.t[:], wv.t[:], AF.Exp, scale=-1.0), [wv], [wv])
                    q_th = wv
                    for qi, Q in enumerate((q_wk, q_rf, q_wi, q_th)):
                        p = psS.next()

                        def f(e, p=p, Q=Q):
                            for c in range(32):
                                ins = e.transpose(p.t[0:64, c * 8:(c + 1) * 8], Q.t[0:8, c * 64:(c + 1) * 64],
                                                  ident.t[0:8, 0:8])
                            return ins
                        S.op('pe', f, reads=[Q, ident], writes=[p])
                        S.op('act', lambda e, p=p, qi=qi: e.copy(cols.t[0:64, qi * 256:(qi + 1) * 256], p.t[0:64, 0:256]),
                             reads=[p], writes=[cols])
                    p = psS.next()

                    def f(e, p=p):
                        for h in range(8):
                            ins = e.matmul(p.t[:, h * 32:(h + 1) * 32], selt.t[0:8, h, :], small.t[0:8, 80:112],
                                           start=True, stop=True)
                        return ins
                    S.op('pe', f, reads=[selt, small], writes=[p])
                    S.op('act', lambda e, p=p: e.copy(decb.t[:], p.t[:, 0:256]), reads=[p], writes=[decb])
                    S.barrier()
                for h in range(8):
                    S.op('pool', lambda e, h=h: e.memset(Cx[h].t[:], 0.0), writes=[Cx[h]])
                    S.op('pool', lambda e, h=h: e.memset(Cb[h].t[:], 0.0), writes=[Cb[h]])
                for c in range(C['nch']):
                    r0 = s * 2048 + c * 64
                    vx_ = vx.next()
                    S.dma(vx_.t[:, :, 0:256], VV[r0:r0 + 64, :].rearrange("t (h v) -> t h v", h=8), writes=[vx_])
                    og_ = ogt.next()
                    S.dma(og_.t[:], OG[r0:r0 + 64, :], writes=[og_])
                    yt = Yt.next()
                    csl = slice(c * 64, (c + 1) * 64)
                    for h in range(C['ncellh']):
                        def col(qi):
                            o = qi * 256 + c * 8 + h
                            return cols.t[0:64, o:o + 1]
                        pa = psS.next()
                        S.op('pe', lambda e, pa=pa, h=h: e.matmul(pa.t[0:64, 0:64], qk.t[:, 8 + h, csl], qk.t[:, h, csl],
                                                                  start=True, stop=True), reads=[qk], writes=[pa])
                        a_ = aT.next()
                        S.op('dve', lambda e, pa=pa, a_=a_: e.tensor_tensor(a_.t[:], pa.t[0:64, 0:64], m01.t[:], ALU.mult),
                             reads=[pa, m01], writes=[a_])
                        vp_ = vp.next()
                        S.op('dve', lambda e, vp_=vp_, h=h, vx_=vx_: e.tensor_scalar(
                            vp_.t[:], vx_.t[:, h, :], col(0), None, ALU.mult), reads=[vx_, cols], writes=[vp_])
                        p1 = psP.next()
                        S.op('pe', lambda e, p1=p1, a_=a_, vp_=vp_: e.matmul(p1.t[0:64, 0:257], a_.t[:], vp_.t[:],
                                                                            start=True, stop=True),
                             reads=[a_, vp_], writes=[p1])
                        p2 = psP.next()
                        S.op('pe', lambda e, p2=p2, h=h: e.matmul(p2.t[0:64, 0:257], qk.t[:, h, csl], Cb[h].t[:],
                                                                  start=True, stop=True),
                             reads=[qk, Cb[h]], writes=[p2])
                        p1s_ = p1s.next()
                        S.op('act', lambda e, p1=p1, p1s_=p1s_: e.activation(p1s_.t[:], p1.t[0:64, 0:257], AF.Copy,
                                                                              scale=col(1)),
                             reads=[p1, cols], writes=[p1s_])
                        n_ = num.next()
                        S.op('dve', lambda e, p2=p2, p1s_=p1s_, n_=n_: e.scalar_tensor_tensor(
                            n_.t[:], p2.t[0:64, 0:257], col(2), p1s_.t[:], ALU.mult, ALU.add),
                            reads=[p2, p1s_, cols], writes=[n_])
                        ss = sst.next()

                        S.op('dve', lambda e, n_=n_, ss=ss: e.scalar_tensor_tensor(
                            ss.t[:, 11:12], n_.t[:, 256:257], -1.0, n_.t[:, 256:257], ALU.mult, ALU.max), reads=[n_], writes=[ss])
                        S.op('dve', lambda e, ss=ss: e.tensor_tensor(
                            ss.t[:, 0:1], ss.t[:, 11:12], col(3), ALU.max), reads=[ss, cols], writes=[ss])
                        S.op('dve', lambda e, ss=ss: e.reciprocal(ss.t[:, 1:2], ss.t[:, 0:1]), reads=[ss], writes=[ss])
                        h_ = hn.next()
                        S.op('dve', lambda e, n_=n_, ss=ss, h_=h_: e.tensor_scalar(
                            h_.t[:], n_.t[:, 0:256], ss.t[:, 1:2], None, ALU.mult), reads=[n_, ss], writes=[h_])

                        S.op('dve', lambda e, h_=h_, ss=ss: e.bn_stats(ss.t[:, 2:8], h_.t[:]), reads=[h_, ss], writes=[ss])
                        S.op('dve', lambda e, ss=ss: e.bn_aggr(ss.t[:, 8:10], ss.t[:, 2:8]), reads=[ss], writes=[ss])
                        S.op('pool', lambda e, ss=ss: e.tensor_scalar(ss.t[:, 10:11], ss.t[:, 9:10], 1e-5, None, ALU.add),
                             reads=[ss], writes=[ss])
                        S.op('pool', lambda e, ss=ss: e.tensor_tensor(ss.t[:, 10:11], ss.t[:, 10:11], mhalf.t[0:64, :], ALU.pow),
                             reads=[ss, mhalf], writes=[ss])
                        S.op('dve', lambda e, h_=h_, ss=ss: e.tensor_scalar(
                            h_.t[:], h_.t[:], ss.t[:, 8:9], ss.t[:, 10:11], ALU.subtract, ALU.mult),
                            reads=[h_, ss], writes=[h_])
                        hs = slice(h * 256, (h + 1) * 256)
                        S.op('dve', lambda e, h_=h_: e.tensor_tensor(h_.t[:], h_.t[:], gbc.t[0:64, hs], ALU.mult),
                             reads=[h_, gbc], writes=[h_])
                        S.op('dve', lambda e, h_=h_, yt=yt, og_=og_: e.tensor_tensor(
                            yt.t[:, hs], h_.t[:], og_.t[:, hs], ALU.mult), reads=[h_, og_], writes=[yt])
                        pk = psB.next()
                        S.op('pe', lambda e, pk=pk, h=h: e.transpose(pk.t[0:64, 0:128], qk.t[:, 8 + h, csl], identb.t[:]),
                             reads=[qk, identb], writes=[pk])
                        k_ = kcs.next()
                        S.op('act', lambda e, pk=pk, k_=k_: e.copy(k_.t[:], pk.t[0:64, 0:128]), reads=[pk], writes=[k_])
                        pu = psU.next()
                        S.op('pe', lambda e, pu=pu, k_=k_, vp_=vp_: e.matmul(pu.t[:, 0:257], k_.t[:], vp_.t[:],
                                                                            start=True, stop=True),
                             reads=[k_, vp_], writes=[pu])
                        S.op('dve', lambda e, pu=pu, h=h: e.scalar_tensor_tensor(
                            Cx[h].t[:], Cx[h].t[:], decb.t[:, h * 32 + c: h * 32 + c + 1], pu.t[:, 0:257],
                            ALU.mult, ALU.add), reads=[Cx[h], decb, pu], writes=[Cx[h]])
                        S.op('act', lambda e, h=h: e.copy(Cb[h].t[:], Cx[h].t[:]), reads=[Cx[h]], writes=[Cb[h]])
                    pk = psB.next()

                    def f(e, pk=pk, yt=yt):
                        for k in range(16):
                            ins = e.transpose(pk.t[:, k * 64:(k + 1) * 64], yt.t[:, k * 128:(k + 1) * 128],
                                              identb.t[0:64, 0:64])
                        return ins
                    S.op('pe', f, reads=[yt, identb], writes=[pk])
                    y_ = yT.next()
                    S.op('act', lambda e, pk=pk, y_=y_: e.copy(y_.t[:], pk.t[:, 0:1024].rearrange("p (k t) -> p k t", k=16)),
                         reads=[pk], writes=[y_])
                    S.dma(OT[:, r0:r0 + 64].rearrange("(c p) t -> p c t", p=128), y_.t[:], reads=[y_])
            S.barrier()

    phase_x2xt()
    if upto >= 1:
        phase_attn()
    if upto >= 2:
        phase_proj_ln(8, w_o, OT, x_in, 0, 0, XRa, not C.get('norouter'))
    if upto >= 3:
        phase_moe(0, XRa, XRb, False)
    if upto >= 4:
        phase_mlstm_proj()
    if upto >= 5:
        phase_mlstm_cell()
    if upto >= 6:
        phase_proj_ln(16, w_out, OT, XRb, 0, 1, XRa, True)
    if upto >= 7:
        phase_moe(1, XRa, y_out, True)
    S.barrier()
    gst.close()
    return nc


def make_consts():
    am = np.full((128, 23, 128), NEG, np.float32)
    q = np.arange(128)[:, None]
    k = np.arange(128)[None, :]
    blk = 0
    for g, (win, dil, dmax) in enumerate(((128, 1, 1), (512, 4, 4), (2048, 16, 15))):
        for delta in range(dmax, -1, -1):
            dist = 128 * delta + q - k
            ok = (dist >= 0) & (dist <= win) & (dist % dil == 0)
            am[:, blk, :] = np.where(ok, 0.0, NEG)
            blk += 1
    inv = (500000.0 ** (-np.arange(0, 32, 2, dtype=np.float32) / 32)).astype(np.float32)
    ang = np.arange(2048, dtype=np.float32)[None, :] * np.concatenate([inv, inv])[:, None]
    rope = np.zeros((32, 2, 2048), np.float32)
    rope[:, 0, :] = np.cos(ang)
    sn = np.sin(ang)
    sn[:16] = -sn[:16]
    rope[:, 1, :] = sn
    pm = np.zeros((32, 32), np.float32)
    for i in range(32):
        pm[(i + 16) % 32, i] = 1.0
    mt = np.where(np.arange(64)[:, None] <= np.arange(64)[None, :], 0.0, NEG).astype(np.float32)
    sel = np.zeros((8, 8, 128), np.float32)
    for h in range(8):
        sel[h, h, :] = 1.0
    return {"c_amask": am.reshape(128, 23 * 128), "c_rope": rope, "c_pm": pm, "c_mt": mt, "c_sel": sel}


def make_in_maps(inp, ncores=8):
    f = lambda a: np.ascontiguousarray(a, dtype=np.float32)
    shared = {
        "attn_w_qkv": f(inp["attn_w_qkv"][0]), "attn_w_o": f(inp["attn_w_o"][0]),
        "mlstm_w_in": f(inp["mlstm_w_in"][0]), "mlstm_b_gates": f(inp["mlstm_b_gates"][0].reshape(16, 1)),
        "mlstm_conv_w": f(inp["mlstm_conv_w"][0]), "mlstm_conv_b": f(inp["mlstm_conv_b"][0].reshape(1, 2048)),
        "mlstm_norm_g": f(inp["mlstm_norm_g"][0].reshape(1, 2048)), "mlstm_w_out": f(inp["mlstm_w_out"][0]),
        "ln_mix_g": f(inp["ln_mix_g"]), "ln_mix_b": f(inp["ln_mix_b"]),
        "ln_ffn_g": f(inp["ln_ffn_g"]), "ln_ffn_b": f(inp["ln_ffn_b"]),
        "router_w": f(inp["router_w"]), "router_b": f(inp["router_b"].reshape(1, 16)),
        "moe_w_gate": f(inp["moe_w_gate"]), "moe_w_up": f(inp["moe_w_up"]), "moe_w_down": f(inp["moe_w_down"]),
    }
    shared.update(make_consts())
    x = np.asarray(inp["x"], dtype=np.float32)
    maps = []
    for c in range(ncores):
        m = dict(shared)
        m["x"] = np.ascontiguousarray(x[2 * c:2 * c + 2].reshape(NTOK, D))
        maps.append(m)
    return maps


def kernel(**inputs):
    nc = build()
    maps = make_in_maps(inputs)
    res = run_bass_kernel_spmd(nc, maps, core_ids=list(range(8)))
    out = np.stack([r["y"].reshape(2, 2048, D) for r in res.results], 0).reshape(16, 2048, D)
    return out.astype(np.float32)
```
